# Optimizing a Trainium2 kernel written in Bass

```python
import math
import jax, jax.numpy as jnp
from jax import lax
import numpy as np

D_MODEL = 2048
BATCH = 8
SEQ = 2048
DEPTH = 1

MIX_WIDTH = D_MODEL
CONV_WIDTH = MIX_WIDTH // 2
MLSTM_WIDTH = MIX_WIDTH - CONV_WIDTH
MLSTM_HEADS = 4
MLSTM_HEAD_DIM = MLSTM_WIDTH // MLSTM_HEADS
CONV_K = 3
CHUNK = 64
D_FF = 4 * D_MODEL
EPS = 1e-6
IN_COLS = 3 * CONV_WIDTH + 4 * MLSTM_WIDTH + 2 * MLSTM_HEADS

kernel_name = "hybrid_conv_mlstm_parallel_heads"


def rms_norm(x, g):
    xf = x.astype(jnp.float32)
    y = xf * lax.rsqrt(jnp.mean(xf * xf, axis=-1, keepdims=True) + EPS)
    return (y * g.astype(jnp.float32)).astype(x.dtype)


def short_conv_mixer(cx, cb, cc, conv_w):
    u = cc * cx
    w = conv_w.astype(u.dtype).reshape(CONV_K, 1, CONV_WIDTH)
    y = lax.conv_general_dilated(
        u, w, window_strides=(1,), padding=[(CONV_K - 1, 0)],
        dimension_numbers=("NWC", "WIO", "NWC"),
        feature_group_count=CONV_WIDTH)
    return cb * y


def mlstm_chunkwise(q, k, v, logi, logf):
    b_, s_, h_, d_ = q.shape
    nc = s_ // CHUNK

    def to_chunks(t):
        return t.reshape(b_, nc, CHUNK, h_, d_).transpose(1, 0, 3, 2, 4)

    def gate_chunks(t):
        return t.reshape(b_, nc, CHUNK, h_).transpose(1, 0, 3, 2)

    qc = to_chunks(q * (d_ ** -0.5))
    kc, vc = to_chunks(k), to_chunks(v)
    ic, fc = gate_chunks(logi), gate_chunks(logf)
    causal = jnp.tril(jnp.ones((CHUNK, CHUNK), dtype=bool))

    def step(carry, inp):
        c_st, n_st, m_st = carry
        qj, kj, vj, li, lf = inp
        bcum = jnp.cumsum(lf, axis=-1)
        log_d = bcum[..., :, None] - bcum[..., None, :] + li[..., None, :]
        log_d = jnp.where(causal, log_d, -jnp.inf)
        m_inter = bcum + m_st[..., None]
        m_row = jnp.maximum(m_inter, jnp.max(log_d, axis=-1))
        dmat = jnp.exp(log_d - m_row[..., None])
        scores = jnp.einsum("bhjd,bhsd->bhjs", qj, kj) * dmat
        inter = jnp.exp(m_inter - m_row)
        num = jnp.einsum("bhjs,bhsd->bhjd", scores, vj) + \
            inter[..., None] * jnp.einsum("bhjd,bhde->bhje", qj, c_st)
        den = jnp.sum(scores, axis=-1) + inter * jnp.einsum("bhjd,bhd->bhj", qj, n_st)
        hj = num / jnp.maximum(jnp.abs(den), jnp.exp(-m_row))[..., None]
        b_last = bcum[..., -1]
        log_w = b_last[..., None] - bcum + li
        m_new = jnp.maximum(b_last + m_st, jnp.max(log_w, axis=-1))
        wgt = jnp.exp(log_w - m_new[..., None])
        decay = jnp.exp(b_last + m_st - m_new)
        c_new = decay[..., None, None] * c_st + jnp.einsum("bhs,bhsd,bhse->bhde", wgt, kj, vj)
        n_new = decay[..., None] * n_st + jnp.einsum("bhs,bhsd->bhd", wgt, kj)
        return (c_new, n_new, m_new), hj

    init = (jnp.zeros((b_, h_, d_, d_), jnp.float32),
            jnp.zeros((b_, h_, d_), jnp.float32),
            jnp.zeros((b_, h_), jnp.float32))
    _, hs = lax.scan(step, init, (qc, kc, vc, ic, fc))
    return hs.transpose(1, 0, 3, 2, 4).reshape(b_, s_, h_, d_)


def hybrid_layer(x, g_pre_mix, g_post_mix, g_pre_mlp, g_post_mlp, w_in, conv_w,
                 b_i, b_f, g_head, w_out, w_mlp1, w_mlp2):
    bsz, seq, _ = x.shape
    u = rms_norm(x, g_pre_mix)
    proj = jnp.einsum("bsd,de->bse", u, w_in)
    c0 = 3 * CONV_WIDTH
    cx = proj[..., 0:CONV_WIDTH]
    cb = proj[..., CONV_WIDTH:2 * CONV_WIDTH]
    cc = proj[..., 2 * CONV_WIDTH:c0]
    q = proj[..., c0:c0 + MLSTM_WIDTH]
    k = proj[..., c0 + MLSTM_WIDTH:c0 + 2 * MLSTM_WIDTH]
    v = proj[..., c0 + 2 * MLSTM_WIDTH:c0 + 3 * MLSTM_WIDTH]
    o = proj[..., c0 + 3 * MLSTM_WIDTH:c0 + 4 * MLSTM_WIDTH]
    g0 = c0 + 4 * MLSTM_WIDTH
    i_pre = proj[..., g0:g0 + MLSTM_HEADS]
    f_pre = proj[..., g0 + MLSTM_HEADS:g0 + 2 * MLSTM_HEADS]

    y_conv = short_conv_mixer(cx, cb, cc, conv_w)

    hd = (bsz, seq, MLSTM_HEADS, MLSTM_HEAD_DIM)
    logi = i_pre.astype(jnp.float32) + b_i.astype(jnp.float32)
    logf = jax.nn.log_sigmoid(f_pre.astype(jnp.float32) + b_f.astype(jnp.float32))
    h = mlstm_chunkwise(q.astype(jnp.float32).reshape(hd), k.astype(jnp.float32).reshape(hd),
                        v.astype(jnp.float32).reshape(hd), logi, logf)
    h = rms_norm(h, g_head.reshape(MLSTM_HEADS, MLSTM_HEAD_DIM)).reshape(bsz, seq, MLSTM_WIDTH)
    y_mlstm = (jax.nn.sigmoid(o.astype(jnp.float32)) * h).astype(x.dtype)

    y = jnp.concatenate([y_conv.astype(x.dtype), y_mlstm], axis=-1)
    y = jnp.einsum("bse,ed->bsd", y, w_out)
    x = x + rms_norm(y, g_post_mix)

    z = rms_norm(x, g_pre_mlp)
    z = jnp.square(jax.nn.relu(jnp.einsum("bsd,df->bsf", z, w_mlp1)))
    z = jnp.einsum("bsf,fd->bsd", z, w_mlp2)
    return x + rms_norm(z, g_post_mlp)


def setup_inputs(seed: int = 0) -> dict:
    key = jax.random.key(seed)
    ks = jax.random.split(key, 16)
    L = DEPTH

    def gain(k_, n):
        return 1.0 + 0.02 * jax.random.normal(k_, (L, n), jnp.float32)

    return {
        "x": jax.random.normal(ks[0], (BATCH, SEQ, D_MODEL), jnp.float32),
        "g_pre_mix": gain(ks[1], D_MODEL),
        "g_post_mix": gain(ks[2], D_MODEL),
        "g_pre_mlp": gain(ks[3], D_MODEL),
        "g_post_mlp": gain(ks[4], D_MODEL),
        "w_in": jax.random.normal(ks[5], (L, D_MODEL, IN_COLS), jnp.float32) * D_MODEL ** -0.5,
        "conv_w": jax.random.normal(ks[6], (L, CONV_K, CONV_WIDTH), jnp.float32) * CONV_K ** -0.5,
        "b_i": 0.1 * jax.random.normal(ks[7], (L, MLSTM_HEADS), jnp.float32) - 1.0,
        "b_f": 3.0 + 0.5 * jax.random.normal(ks[8], (L, MLSTM_HEADS), jnp.float32),
        "g_head": gain(ks[9], MLSTM_WIDTH),
        "w_out": jax.random.normal(ks[10], (L, MIX_WIDTH, D_MODEL), jnp.float32) * MIX_WIDTH ** -0.5,
        "w_mlp1": jax.random.normal(ks[11], (L, D_MODEL, D_FF), jnp.float32) * D_MODEL ** -0.5,
        "w_mlp2": jax.random.normal(ks[12], (L, D_FF, D_MODEL), jnp.float32) * D_FF ** -0.5,
    }


def reference(x, g_pre_mix, g_post_mix, g_pre_mlp, g_post_mlp, w_in, conv_w,
              b_i, b_f, g_head, w_out, w_mlp1, w_mlp2):
    for layer in range(DEPTH):
        x = hybrid_layer(x, g_pre_mix[layer], g_post_mix[layer], g_pre_mlp[layer],
                         g_post_mlp[layer], w_in[layer], conv_w[layer], b_i[layer],
                         b_f[layer], g_head[layer], w_out[layer], w_mlp1[layer],
                         w_mlp2[layer])
    return x
```

```python
import math
from contextlib import ExitStack

import numpy as np
import ml_dtypes
import concourse.bass as bass
import concourse.mybir as mybir
from concourse.bass_utils import run_bass_kernel_spmd

F32 = mybir.dt.float32
BF16 = mybir.dt.bfloat16
AF = mybir.ActivationFunctionType
ALU = mybir.AluOpType
AX = mybir.AxisListType

S = 2048
D = 2048
T = 512
NB = 4
DI = 7176
DFF = 8192
EPS = 1e-6
NSLOT = 4
USE_SCRATCH = False
ENGS = ("pe", "act", "dve", "pool", "sp")


class Op:
    __slots__ = ("eng", "fn", "deps", "sig", "sem", "val", "dma", "name")

    def __init__(self, eng, fn, dma, name):
        self.eng = eng
        self.fn = fn
        self.deps = []
        self.sig = False
        self.sem = None
        self.val = 0
        self.dma = dma
        self.name = name


class Prog:
    def __init__(self, nc):
        self.nc = nc
        self.ops = {e: [] for e in ENGS}
        self.last_w = {}
        self.readers = {}
        self.dma_sems = {}
        self.eng_sem = {}
        self.all = []
        self.bank_acc = {}

    def add(self, eng, fn, reads=(), writes=(), dma=None, name=""):
        op = Op(eng, fn, dma, name)
        deps = []
        banks = set(k[1] for k in list(reads) + list(writes) if isinstance(k, tuple) and k[0] == "ps")
        reads = [k for k in reads if not (isinstance(k, tuple) and k[0] == "ps")]
        writes = [k for k in writes if not (isinstance(k, tuple) and k[0] == "ps")]
        for bnk in sorted(banks):
            acc = self.bank_acc.setdefault(bnk, {})
            for e2, o2 in acc.items():
                if e2 != eng:
                    deps.append((o2, "bank"))
            acc[eng] = op
        for k in reads:
            w = self.last_w.get(k)
            if w is not None:
                deps.append((w, "raw"))
        for k in writes:
            w = self.last_w.get(k)
            if w is not None:
                deps.append((w, "waw"))
            for r in self.readers.get(k, ()):
                deps.append((r, "war"))
        seen = set()
        for (d, kind) in deps:
            if d is op:
                continue
            if d.eng == eng and d.dma is None and op.dma is None and eng == "pe":
                continue
            if id(d) in seen:
                continue
            seen.add(id(d))
            op.deps.append(d)
        for k in reads:
            self.readers.setdefault(k, []).append(op)
        for k in writes:
            self.last_w[k] = op
            self.readers[k] = []
        self.ops[eng].append(op)
        self.all.append(op)
        return op

    def emit(self, stack):
        nc = self.nc
        for e in ENGS:
            self.eng_sem[e] = stack.enter_context(nc.semaphore("es_" + e))
        nk = 0
        for op in self.all:
            if op.dma is not None and op.dma not in self.dma_sems:
                self.dma_sems[op.dma] = [stack.enter_context(nc.semaphore("ds_%d" % nk)), 0]
                nk += 1
        for op in self.all:
            for d in op.deps:
                d.sig = True
        cnt = {e: 0 for e in ENGS}
        for op in self.all:
            if op.dma is not None:
                s = self.dma_sems[op.dma]
                s[1] += 16
                op.sem = s[0]
                op.val = s[1]
                op.sig = True
            elif op.sig:
                cnt[op.eng] += 1
                op.sem = self.eng_sem[op.eng]
                op.val = cnt[op.eng]
        self.max_vals = dict(cnt)
        block = stack.enter_context(nc.Block())

        def run(engname, e):
            waited = {}
            for op in self.ops[engname]:
                need = {}
                for d in op.deps:
                    key = d.sem.num
                    if waited.get(key, 0) >= d.val:
                        continue
                    if key not in need or need[key][1] < d.val:
                        need[key] = (d.sem, d.val)
                for key, (sem, val) in need.items():
                    e.wait_ge(sem, val)
                    waited[key] = val
                ins = op.fn(e)
                if op.sig:
                    assert ins is not None, op.name
                    ins.then_inc(op.sem, 16 if op.dma is not None else 1)
            if engname == "sp":
                for k, (sem, val) in self.dma_sems.items():
                    if waited.get(sem.num, 0) < val:
                        e.wait_ge(sem, val)

        @block.sync
        def _(e):
            run("sp", e)

        @block.tensor
        def _(e):
            run("pe", e)

        @block.scalar
        def _(e):
            run("act", e)

        @block.vector
        def _(e):
            run("dve", e)

        @block.gpsimd
        def _(e):
            run("pool", e)


def pg(off, n):
    return [("R", p) for p in range(off // 1024, (off + n - 1) // 1024 + 1)]


def psk(b, q=None):
    if q is None:
        return [("ps", b, i) for i in range(4)]
    return [("ps", b, q)]


def build_nc(nblk=NB, dbg=False):
    nc = bass.Bass("TRN2", target_bir_lowering=False)

    def din(name, shape, dt=F32):
        return nc.dram_tensor(name, shape, dt, kind="ExternalInput").ap()

    def dout(name, shape, dt=F32):
        return nc.dram_tensor(name, shape, dt, kind="ExternalOutput").ap()

    x = din("x", [S, D])
    w_in = din("w_in", [D, DI])
    w_out = din("w_out", [D, D])
    w1 = din("w_mlp1", [D, DFF])
    w2 = din("w_mlp2", [DFF, D])
    gcol_d = din("gcol", [128, 32])
    gpost_d = din("gpost", [128, 2, D])
    ghead_d = din("ghead", [128, 1024])
    convw_d = din("convw", [128, 8, 3])
    bif_d = din("bif", [4, 2])
    ident_d = din("ident", [128, 128], BF16)
    cmask_d = din("cmask", [128, 128])
    sel_d = din("sel", [4, 512])
    i4_d = din("i4", [4, 4])
    out = dout("out", [S, D])
    NUNIT = 200 if USE_SCRATCH else 1
    scratch = nc.dram_tensor("wscr", [NUNIT, 128, 2048], BF16, kind="Internal").ap()
    if dbg:
        d_uT = dout("d_uT", [128, 16 * 512], BF16)
        d_gates = dout("d_gates", [4, 4 * 512])
        d_cols = dout("d_cols", [128, 32 + 16])
        d_yT = dout("d_yT", [128, 16 * 512], BF16)
        d_x1 = dout("d_x1", [128, 4 * D])
        d_qk = dout("d_qk", [128, 3 * 4 * 2 * 512], BF16)
        d_v = dout("d_v", [128, 4 * 4 * 258], BF16)

    with ExitStack() as st:
        P = Prog(nc)

        def sb(name, shape, dt=F32):
            return st.enter_context(nc.sbuf_tensor("s_" + name, shape, dt))

        xres = sb("xres", [128, 4, D])
        bufA = sb("bufA", [128, 16, T], BF16)
        R = sb("R", [128, 16384])
        y2s = sb("y2s", [128, 4, D])
        ring = sb("ring", [128, NSLOT, 2048], BF16)
        gpost = sb("gpost", [128, 2, D])
        ghead = sb("ghead", [128, 1024])
        G32 = sb("G32", [128, 4, 2, 257])
        Gbf = sb("Gbf", [128, 4, 2, 258], BF16)
        Gs = sb("Gs", [128, 2, 2, 257])
        halo = sb("halo", [128, 8, 2])
        convw = sb("convw", [128, 8, 3])
        gcol = sb("gcol", [128, 32])
        Wg = sb("Wg", [128, 16, 8], BF16)
        idt = sb("idt", [128, 128], BF16)
        cmask = sb("cmask", [128, 128])
        selt = sb("selt", [4, 512])
        i4t = sb("i4t", [4, 4])
        bif = sb("bif", [4, 2])
        negbf = sb("negbf", [4, 1])
        zer4 = sb("zer4", [4, 512])
        m_in = sb("m_in", [4, 1])
        mup = sb("mup", [4, 4])
        dec = sb("dec", [4, 4])
        SpT = sb("SpT", [128, 2, 128], BF16)
        ktil = sb("ktil", [128, 2, 256], BF16)
        hn = sb("hn", [128, 2, 256], BF16)
        rbuf = sb("rbuf", [128, 2, T], BF16)
        junk_s = sb("junk_s", [128, T], BF16)
        cols = sb("cols", [128, 4, 8])
        decbc = sb("decbc", [128, 16])
        stt = sb("stt", [128, 64])
        ps = [st.enter_context(nc.psum_tensor("ps%d" % i, [128, 512], F32)) for i in range(8)]

        hT = R[:, :].bitcast(BF16).rearrange("p (f t) -> p f t", f=64)
        yT = R[:, 0:4096].bitcast(BF16).rearrange("p (k t) -> p k t", k=16)
        uT = bufA
        qT = R[:, 4096:6144].bitcast(BF16).rearrange("p (h c t) -> p h c t", h=4, c=2)
        kT = R[:, 6144:8192].bitcast(BF16).rearrange("p (h c t) -> p h c t", h=4, c=2)
        soT = R[:, 8192:10240].bitcast(BF16).rearrange("p (h c t) -> p h c t", h=4, c=2)
        vext = R[:, 10240:12304].bitcast(BF16).rearrange("p (m h e) -> p m h e", m=4, h=4)
        VOFF = 40960
        UCOFF = 50176
        ucb = [R[:, 12544 + i * 514: 12544 + (i + 1) * 514] for i in range(2)]
        IBOFF = 55296
        interbc = R[:, 13824:15872].rearrange("p (h t) -> p h t", h=4)

        def k_uT(k):
            return [("A", k)]

        def k_qT(h, c):
            return [("R", 16 + 2 * h + c)]

        def k_kT(h, c):
            return [("R", 24 + 2 * h + c)]

        def k_soT(h, c):
            return [("R", 32 + 2 * h + c)]

        def k_v(m, h):
            return pg(VOFF + (m * 4 + h) * 516, 516)

        def k_uc(i):
            return pg(UCOFF + i * 2056, 2056)

        def k_ib(h):
            return pg(IBOFF + h * 2048, 2048)

        def ubf(m):
            return y2s[:, m, 0:1024].bitcast(BF16)

        def junkA(m):
            return y2s[:, m, 1024:2048].bitcast(BF16)

        def k_y2(m, n):
            if n < 2:
                return [("y2lo", m)]
            return [("y2hi", m, n - 2)]

        def k_y2all(m):
            return [("y2lo", m), ("y2hi", m, 0), ("y2hi", m, 1)]

        cx_s = [y2s[:, i, 1024:1536] for i in range(2)]
        ycv = [y2s[:, i, 1536:2048] for i in range(2)]
        GA = y2s[0:4, 2, 1024:1536]
        GB = y2s[0:4, 2, 1536:2048]
        GC = y2s[0:4, 3, 1024:1536]
        GD = y2s[0:4, 3, 1536:2048]
        kGA, kGB, kGC, kGD = [("y2hi", 2, 0)], [("y2hi", 2, 1)], [("y2hi", 3, 0)], [("y2hi", 3, 1)]

        ring_ctr = [0]

        def ring_next():
            s = ring_ctr[0] % NSLOT
            ring_ctr[0] += 1
            return s

        def ring_fm(s):
            return ring[:, s, :].rearrange("p (kk c) -> p kk c", kk=8)

        def ring_tm(s):
            return ring[:, s, :].rearrange("p (kk c) -> p kk c", kk=4)

        def ld(dst, src, key):
            P.add("sp", lambda e: e.dma_start(out=dst, in_=src), writes=[key], dma=key)

        ld(gpost[:], gpost_d, "gpost")
        ld(ghead[:], ghead_d, "ghead")
        ld(convw[:], convw_d, "convw")
        ld(gcol[:], gcol_d, "gcol")
        ld(idt[:], ident_d, "idt")
        ld(cmask[:], cmask_d, "cmask")
        ld(selt[:], sel_d, "selt")
        ld(i4t[:], i4_d, "i4t")
        ld(bif[:], bif_d, "bif")
        P.add("pool", lambda e: e.dma_start(
            out=Wg[:], in_=w_in[:, 7168:7176].rearrange("(k p) c -> p k c", p=128)), writes=["Wg"], dma="Wg")
        P.add("dve", lambda e: e.memset(G32[:], 0.0), writes=["G32_%d" % h for h in range(4)])
        P.add("dve", lambda e: e.memset(Gbf[:], 0.0), writes=["Gbf_%d" % h for h in range(4)])
        P.add("dve", lambda e: e.memset(halo[:], 0.0), writes=["halo"])
        P.add("dve", lambda e: e.memset(zer4[:], 0.0), writes=["zer4"])
        P.add("dve", lambda e: e.memset(m_in[:], 0.0), writes=["m_in"])
        P.add("dve", lambda e: e.tensor_scalar(out=negbf[:], in0=bif[:, 1:2], scalar1=-1.0, scalar2=None, op0=ALU.mult),
              reads=["bif"], writes=["negbf"])

        def rstd_ops(ss_ap, rs_ap, n, kss, krs):
            P.add("act", lambda e: e.activation(out=rs_ap, in_=ss_ap, func=AF.Sqrt, scale=1.0 / n, bias=EPS),
                  reads=kss, writes=krs)
            P.add("dve", lambda e: e.reciprocal(out=rs_ap, in_=rs_ap), reads=krs, writes=krs)

        def norm_pre_sq(m):
            ss = stt[:, m:m + 1]
            P.add("act", lambda e: e.activation(out=junkA(m), in_=xres[:, m, :], func=AF.Square, accum_out=ss),
                  reads=[("xres", m)], writes=[("y2hi", m, 0), ("y2hi", m, 1), ("ss", m)])

        def norm_pre_rs(m):
            rstd_ops(stt[:, m:m + 1], stt[:, 4 + m:5 + m], D, [("ss", m)], [("rs", m)])

        def norm_pre_scale(m):
            rs = stt[:, 4 + m:5 + m]
            if m % 2 == 0:
                P.add("dve", lambda e: e.tensor_scalar(out=ubf(m), in0=xres[:, m, :], scalar1=rs, scalar2=None,
                                                       op0=ALU.mult),
                      reads=[("xres", m), ("rs", m)], writes=[("y2lo", m)])
            else:
                P.add("act", lambda e: e.activation(out=ubf(m), in_=xres[:, m, :], func=AF.Copy, scale=rs),
                      reads=[("xres", m), ("rs", m)], writes=[("y2lo", m)])

        def norm_pre(m):
            norm_pre_sq(m)
            norm_pre_rs(m)
            norm_pre_scale(m)

        def norm_tr(dstT, dkey, goff, mlist, srcf=None, skeyf=None, bank0=0):
            if srcf is None:
                srcf = ubf
                skeyf = lambda m: [("y2lo", m)]
            m0 = mlist[0]
            nm = len(mlist)
            for k in range(16):
                bb = bank0 + k % 2
                pT = ps[bb][:, 0:256].bitcast(BF16)
                for j, m in enumerate(mlist):
                    P.add("pe", lambda e, k=k, m=m, j=j, pT=pT: e.transpose(
                        out=pT[:, j * 128:(j + 1) * 128], in_=srcf(m)[:, k * 128:(k + 1) * 128], identity=idt[:]),
                        reads=skeyf(m) + ["idt"], writes=psk(bb))
                gc = gcol[:, goff + k:goff + k + 1]
                dst = dstT[:, k, m0 * 128:(m0 + nm) * 128]
                src = pT[:, 0:nm * 128]
                if k % 2 == 0:
                    P.add("dve", lambda e, dst=dst, src=src, gc=gc: e.tensor_scalar(
                        out=dst, in0=src, scalar1=gc, scalar2=None, op0=ALU.mult),
                        reads=psk(bb) + ["gcol"], writes=dkey(k))
                else:
                    P.add("act", lambda e, dst=dst, src=src, gc=gc: e.activation(
                        out=dst, in_=src, func=AF.Copy, scale=gc),
                        reads=psk(bb) + ["gcol"], writes=dkey(k))

        def norm_T(dstT, dkey, goff, tag):
            for m in range(4):
                norm_pre(m)
            norm_tr(dstT, dkey, goff, (0, 1, 2, 3))

        def ubfN(m):
            return y2s[:, m, 1024:2048].bitcast(BF16)

        def k_ubfN(m):
            return [("y2hi", m, 0), ("y2hi", m, 1)]

        def stageA_part1(bn, m):
            i = m % 2
            r0 = bn * T + m * 128
            L = y2s[:, 2 * i:2 * i + 2, 0:1024]
            kL = [("y2lo", 2 * i), ("y2lo", 2 * i + 1)]
            u3 = ubfN(m).rearrange("p (a c) -> p a c", a=2)
            ss = stt[:, 56 + m:57 + m]
            rs = stt[:, 60 + m:61 + m]
            P.add("sp", lambda e: e.dma_start(out=L, in_=x[r0:r0 + 128, :].rearrange("p (a c) -> p a c", a=2)),
                  writes=kL, dma=("xpre", i))
            P.add("act", lambda e: e.activation(out=u3, in_=L, func=AF.Square, accum_out=ss),
                  reads=kL, writes=k_ubfN(m) + [("ssN", m)])
            rstd_ops(ss, rs, D, [("ssN", m)], [("rsN", m)])
            P.add("dve", lambda e: e.tensor_scalar(out=u3, in0=L, scalar1=rs, scalar2=None, op0=ALU.mult),
                  reads=kL + [("rsN", m)], writes=k_ubfN(m))

        def stageA_part2(bank0):
            norm_tr(bufA, k_uT, 0, (0, 1, 2, 3), srcf=ubfN, skeyf=k_ubfN, bank0=bank0)

        fm_ctr = [0]
        scr_idx = {}
        scr_ctr = [0]
        n_new = [0]
        cur_b = [0]

        def load_unit(s, wname, W, kind, r0, c0):
            key = (wname, kind, r0, c0)
            if key in scr_idx:
                idx = scr_idx[key]
                P.add("sp", lambda e: e.dma_start(out=ring[:, s, :], in_=scratch[idx]),
                      reads=[("scr", idx)], writes=[("ring", s)], dma=("ring", s))
                return
            if kind == "fm":
                dst = ring_fm(s)
                src = W[r0:r0 + 1024, c0:c0 + 256].rearrange("(kk p) c -> p kk c", p=128)
            else:
                dst = ring_tm(s)
                src = W[r0:r0 + 512, c0:c0 + 512].rearrange("(kk p) c -> p kk c", p=128)
            P.add("pool", lambda e: e.dma_start(out=dst, in_=src), writes=[("ring", s)], dma=("ring", s))
            n_new[0] += 1
            if USE_SCRATCH and (cur_b[0] >= 1 or n_new[0] % 2 == 0):
                idx = scr_ctr[0]
                scr_ctr[0] += 1
                assert idx < NUNIT
                scr_idx[key] = idx
                P.add("sp", lambda e: e.dma_start(out=scratch[idx], in_=ring[:, s, :]),
                      reads=[("ring", s)], writes=[("scr", idx)], dma=("scrst", s))

        def fm_proj(W, col0, src, skey, evac, banks=None, wname="w_in"):
            if banks is None:
                par = fm_ctr[0] % 2
                fm_ctr[0] += 1
                banks = (4 + 2 * par, 5 + 2 * par)
            for half in range(2):
                s = ring_next()
                load_unit(s, wname, W, "fm", half * 1024, col0)
                for kk in range(8):
                    k = half * 8 + kk
                    for ci in range(2):
                        P.add("pe", lambda e, s=s, kk=kk, k=k, ci=ci: e.matmul(
                            ps[banks[ci]][:, :], lhsT=ring_fm(s)[:, kk, ci * 128:(ci + 1) * 128], rhs=src[:, k, :],
                            start=(k == 0), stop=(k == 15)),
                            reads=[("ring", s)] + skey(k), writes=psk(banks[ci]))
            for ci in range(2):
                evac(ci, ps[banks[ci]], psk(banks[ci]))

        def tm_proj(W, col0, src, skey, nkt, evac, mlist=(0, 1, 2, 3), wname="w_in", bank0=0):
            for u in range(nkt // 4):
                s = ring_next()
                load_unit(s, wname, W, "tm", u * 512, col0)
                for kk in range(4):
                    k = u * 4 + kk
                    for m in mlist:
                        P.add("pe", lambda e, s=s, kk=kk, k=k, m=m: e.matmul(
                            ps[bank0 + m][:, :], lhsT=src[:, k, m * 128:(m + 1) * 128], rhs=ring_tm(s)[:, kk, :],
                            start=(k == 0), stop=(k == nkt - 1)),
                            reads=[("ring", s)] + skey(k), writes=psk(bank0 + m))
            for m in mlist:
                evac(m, ps[bank0 + m], psk(bank0 + m))

        def post_evac(n, ssq_off, gi):
            def ev(m, bank, bkey):
                dst = y2s[:, m, n * 512:(n + 1) * 512]
                sq = stt[:, ssq_off + m * 4 + n: ssq_off + m * 4 + n + 1]
                P.add("act", lambda e: e.activation(out=junk_s[:], in_=bank[:, :], func=AF.Square, accum_out=sq),
                      reads=bkey, writes=["junk_s", ("ssq", m, n)])
                P.add("dve", lambda e: e.tensor_tensor(out=dst, in0=bank[:, :], in1=gpost[:, gi, n * 512:(n + 1) * 512],
                                                       op=ALU.mult),
                      reads=bkey + ["gpost"], writes=k_y2(m, n))
            return ev

        def post_rstd(m, ssq_off):
            ssv = stt[:, ssq_off + m * 4: ssq_off + m * 4 + 4]
            ss = stt[:, 8 + m:9 + m]
            rs = stt[:, 12 + m:13 + m]
            P.add("dve", lambda e: e.reduce_sum(out=ss, in_=ssv, axis=AX.X),
                  reads=[("ssq", m, n) for n in range(4)], writes=[("ssp", m)])
            rstd_ops(ss, rs, D, [("ssp", m)], [("rsp", m)])
            return rs

        for b in range(nblk):
            t0 = b * T
            cur_b[0] = b
            mq = "sp" if (b == 0 or not USE_SCRATCH) else "pool"
            for m in range(4):
                r0 = t0 + m * 128
                P.add(mq, lambda e, m=m, r0=r0: e.dma_start(out=xres[:, m, :], in_=x[r0:r0 + 128, :]),
                      writes=[("xres", m)], dma=("xin", m))
            if b == 0:
                for m in range(4):
                    stageA_part1(0, m)
                stageA_part2(0)
            if dbg and b == 0:
                P.add("sp", lambda e: e.dma_start(out=d_uT, in_=bufA[:, :, :].rearrange("p k t -> p (k t)")),
                      reads=[("A", k) for k in range(16)], dma="d_uT")

            psi = ps[2][0:4, :]
            psf = ps[3][0:4, :]
            for gi, pso, bk in ((0, psi, 2), (1, psf, 3)):
                for k in range(16):
                    P.add("pe", lambda e, k=k, gi=gi, pso=pso: e.matmul(
                        pso, lhsT=Wg[:, k, gi * 4:(gi + 1) * 4], rhs=uT[:, k, :], start=(k == 0), stop=(k == 15)),
                        reads=k_uT(k) + ["Wg"], writes=psk(bk))
            P.add("act", lambda e: e.activation(out=GA, in_=psf, func=AF.Exp, scale=-1.0, bias=negbf[:, 0:1]),
                  reads=psk(3) + ["negbf"], writes=kGA)
            P.add("act", lambda e: e.activation(out=GA, in_=GA, func=AF.Ln, scale=1.0, bias=1.0),
                  reads=kGA, writes=kGA)
            P.add("dve", lambda e: e.tensor_tensor_scan(out=GB, data0=GA, data1=zer4[:], initial=0.0,
                                                        op0=ALU.add, op1=ALU.add),
                  reads=kGA + ["zer4"], writes=kGB)
            P.add("dve", lambda e: e.scalar_tensor_tensor(out=GA, in0=psi, scalar=bif[:, 0:1], in1=GB,
                                                          op0=ALU.add, op1=ALU.add),
                  reads=psk(2) + kGB + ["bif"], writes=kGA)
            P.add("dve", lambda e: e.tensor_tensor_scan(out=GC, data0=GA, data1=GA, initial=m_in[:, 0:1],
                                                        op0=ALU.max, op1=ALU.max),
                  reads=kGA + ["m_in"], writes=kGC)
            P.add("dve", lambda e: e.tensor_copy(out=mup[:, 0:1], in_=m_in[:, 0:1]), reads=["m_in"], writes=["mup0"])
            P.add("dve", lambda e: e.tensor_copy(out=mup[:, 1:4], in_=GC[:, 127:384:128]), reads=kGC, writes=["mup1"])
            P.add("dve", lambda e: e.tensor_tensor(out=m_in[:, 0:1], in0=GC[:, 511:512], in1=GB[:, 511:512],
                                                   op=ALU.subtract),
                  reads=kGC + kGB, writes=["m_in"])
            mup_bc = mup[:, :].rearrange("p (c o) -> p c o", o=1).to_broadcast([4, 4, 128])

            def v3(ap):
                return ap.rearrange("p (c t) -> p c t", c=4)
            P.add("dve", lambda e: e.tensor_tensor(out=v3(GD), in0=mup_bc, in1=v3(GC), op=ALU.subtract),
                  reads=["mup0", "mup1"] + kGC, writes=kGD)
            P.add("act", lambda e: e.activation(out=GD, in_=GD, func=AF.Exp, scale=1.0, bias=math.log(1.0 / 16.0)),
                  reads=kGD, writes=kGD)
            P.add("dve", lambda e: e.tensor_tensor(out=dec[:, :], in0=mup[:, :], in1=GC[:, 127:512:128],
                                                   op=ALU.subtract),
                  reads=["mup0", "mup1"] + kGC, writes=["dec"])
            P.add("act", lambda e: e.activation(out=dec[:, :], in_=dec[:, :], func=AF.Exp), reads=["dec"], writes=["dec"])
            P.add("dve", lambda e: e.tensor_tensor(out=GB, in0=GB, in1=GC, op=ALU.subtract),
                  reads=kGB + kGC, writes=kGB)
            P.add("act", lambda e: e.activation(out=GB, in_=GB, func=AF.Exp), reads=kGB, writes=kGB)
            P.add("dve", lambda e: e.tensor_tensor(out=v3(GA), in0=v3(GA), in1=mup_bc, op=ALU.subtract),
                  reads=["mup0", "mup1"] + kGA, writes=kGA)
            P.add("act", lambda e: e.activation(out=GA, in_=GA, func=AF.Exp), reads=kGA, writes=kGA)
            if dbg and b == 0:
                for i, (g, kk) in enumerate(((GA, kGA), (GB, kGB), (GC, kGC), (GD, kGD))):
                    P.add("sp", lambda e, i=i, g=g: e.dma_start(out=d_gates[:, i * 512:(i + 1) * 512], in_=g),
                          reads=kk, dma=("d_g", i))
            conv_calls = []
            for cp in range(4):
                def ev_x(ci, bank, bkey):
                    P.add("act", lambda e: e.activation(out=cx_s[ci], in_=bank[:, :], func=AF.Copy),
                          reads=bkey, writes=[("y2hi", ci, 0)])

                def ev_c(ci, bank, bkey, cp=cp):
                    cb = cp * 2 + ci
                    uc = ucb[ci]
                    P.add("dve", lambda e: e.tensor_copy(out=uc[:, 0:2], in_=halo[:, cb, :]),
                          reads=["halo"], writes=k_uc(ci))
                    P.add("dve", lambda e: e.tensor_tensor(out=uc[:, 2:514], in0=bank[:, :], in1=cx_s[ci], op=ALU.mult),
                          reads=bkey + [("y2hi", ci, 0)], writes=k_uc(ci))
                    P.add("dve", lambda e: e.tensor_copy(out=halo[:, cb, :], in_=uc[:, 512:514]),
                          reads=k_uc(ci), writes=["halo"])
                    P.add("act", lambda e: e.activation(out=ycv[ci], in_=uc[:, 0:512], func=AF.Copy,
                                                        scale=convw[:, cb, 0:1]),
                          reads=k_uc(ci) + ["convw"], writes=[("y2hi", ci, 1)])
                    for tap in (1, 2):
                        P.add("dve", lambda e, tap=tap: e.scalar_tensor_tensor(
                            out=ycv[ci], in0=uc[:, tap:tap + 512], scalar=convw[:, cb, tap:tap + 1], in1=ycv[ci],
                            op0=ALU.mult, op1=ALU.add),
                            reads=k_uc(ci) + ["convw", ("y2hi", ci, 1)], writes=[("y2hi", ci, 1)])

                def ev_b(ci, bank, bkey, cp=cp):
                    cb = cp * 2 + ci
                    P.add("dve", lambda e: e.tensor_tensor(out=yT[:, cb, :], in0=bank[:, :], in1=ycv[ci], op=ALU.mult),
                          reads=bkey + [("y2hi", ci, 1)], writes=[("R", cb)])
                conv_calls.append(lambda cp=cp, ev=ev_x: fm_proj(w_in, cp * 256, uT, k_uT, ev, banks=(6, 7)))
                conv_calls.append(lambda cp=cp, ev=ev_c: fm_proj(w_in, 2048 + cp * 256, uT, k_uT, ev, banks=(6, 7)))
                conv_calls.append(lambda cp=cp, ev=ev_b: fm_proj(w_in, 1024 + cp * 256, uT, k_uT, ev, banks=(6, 7)))

            P.add("dve", lambda e: e.memset(vext[:, :, :, 256:258], 1.0), writes=pg(VOFF, 8256))
            for half in range(2):
                def ev_v(m, bank, bkey, half=half):
                    dst = vext[:, m, 2 * half:2 * half + 2, 0:256]
                    src = bank[:, :].rearrange("p (h e) -> p h e", h=2)
                    kk = k_v(m, 2 * half) + k_v(m, 2 * half + 1)
                    if m % 2 == 0:
                        P.add("dve", lambda e: e.tensor_copy(out=dst, in_=src), reads=bkey, writes=kk)
                    else:
                        P.add("act", lambda e: e.activation(out=dst, in_=src, func=AF.Copy), reads=bkey, writes=kk)
                tm_proj(w_in, 5120 + half * 512, uT, k_uT, 16, ev_v, bank0=4 * half)
            o_calls = []
            q_calls = []
            for h in range(4):
                def ev_q(ci, bank, bkey, h=h):
                    P.add("dve", lambda e: e.tensor_tensor(out=qT[:, h, ci, :], in0=bank[:, :], in1=interbc[:, h, :],
                                                           op=ALU.mult),
                          reads=bkey + k_ib(h), writes=k_qT(h, ci))

                def ev_k(ci, bank, bkey, h=h):
                    P.add("act", lambda e: e.activation(out=kT[:, h, ci, :], in_=bank[:, :], func=AF.Copy),
                          reads=bkey, writes=k_kT(h, ci))

                def ev_o(ci, bank, bkey, h=h):
                    P.add("act", lambda e: e.activation(out=soT[:, h, ci, :], in_=bank[:, :], func=AF.Sigmoid),
                          reads=bkey, writes=k_soT(h, ci))
                q_calls.append(lambda h=h, ev=ev_q: fm_proj(w_in, 3072 + h * 256, uT, k_uT, ev))
                fm_proj(w_in, 4096 + h * 256, uT, k_uT, ev_k)
                o_calls.append(lambda h=h, ev=ev_o: fm_proj(w_in, 6144 + h * 256, uT, k_uT, ev, banks=(6, 7)))
            for h in range(4):
                P.add("pe", lambda e, h=h: e.matmul(ps[4 + h][:, :], lhsT=selt[:, h * 128:(h + 1) * 128], rhs=GD,
                                                    start=True, stop=True),
                      reads=["selt"] + kGD, writes=psk(4 + h))
                if h % 2 == 0:
                    P.add("dve", lambda e, h=h: e.tensor_copy(out=interbc[:, h, :], in_=ps[4 + h][:, :]),
                          reads=psk(4 + h), writes=k_ib(h))
                else:
                    P.add("act", lambda e, h=h: e.activation(out=interbc[:, h, :], in_=ps[4 + h][:, :], func=AF.Copy),
                          reads=psk(4 + h), writes=k_ib(h))
            for m in range(4):
                P.add("pe", lambda e, m=m: e.matmul(ps[2][:, m * 8:m * 8 + 4], lhsT=GA[:, m * 128:(m + 1) * 128],
                                                    rhs=i4t[:, :], start=True, stop=True),
                      reads=kGA + ["i4t"], writes=psk(2))
                P.add("pe", lambda e, m=m: e.matmul(ps[2][:, m * 8 + 4:m * 8 + 8], lhsT=GB[:, m * 128:(m + 1) * 128],
                                                    rhs=i4t[:, :], start=True, stop=True),
                      reads=kGB + ["i4t"], writes=psk(2))
            P.add("dve", lambda e: e.tensor_copy(out=cols[:, :, :], in_=ps[2][:, 0:32].rearrange("p (m c) -> p m c", m=4)),
                  reads=psk(2), writes=["cols"])
            for h in range(4):
                P.add("pe", lambda e, h=h: e.matmul(ps[3][:, h * 4:(h + 1) * 4], lhsT=selt[:, h * 128:(h + 1) * 128],
                                                    rhs=dec[:, :], start=True, stop=True),
                      reads=["selt", "dec"], writes=psk(3))
            P.add("dve", lambda e: e.tensor_copy(out=decbc[:, :], in_=ps[3][:, 0:16]), reads=psk(3), writes=["decbc"])
            if dbg and b == 0:
                P.add("sp", lambda e: e.dma_start(out=d_cols[:, 0:32], in_=cols[:, :, :].rearrange("p m c -> p (m c)")),
                      reads=["cols"], dma="d_c1")
                P.add("sp", lambda e: e.dma_start(out=d_cols[:, 32:48], in_=decbc[:, :]), reads=["decbc"], dma="d_c2")

            for qc in q_calls:
                qc()
            if dbg and b == 0:
                P.add("sp", lambda e: e.dma_start(out=d_qk, in_=R[:, 4096:10240].bitcast(BF16)),
                      reads=[("R", p) for p in range(16, 40)], dma="d_qk")
                P.add("sp", lambda e: e.dma_start(out=d_v, in_=R[:, 10240:12304].bitcast(BF16)),
                      reads=pg(VOFF, 8256), dma="d_v")

            def ml_step(m, h, stage):
                ms = slice(m * 128, (m + 1) * 128)
                par = h % 2
                pST = ps[0][:, 0:128]
                pKT = ps[1][:, 0:128].bitcast(BF16)
                pHT = ps[2][:, 0:128].bitcast(BF16)
                pN = ps[3][:, 0:257]
                pP = [ps[4][:, 0:257], ps[5][:, 0:257]]
                w1c = cols[:, m, h:h + 1]
                enc = cols[:, m, 4 + h:5 + h]
                dcc = decbc[:, h * 4 + m:h * 4 + m + 1]
                so = 16 + par * 4
                s1 = stt[:, so:so + 1]
                s2 = stt[:, so + 1:so + 2]
                ssn = stt[:, so + 2:so + 3]
                kst = [("mls", par)]
                if stage == 1:
                    for dc in range(2):
                        P.add("pe", lambda e, dc=dc, h=h, ms=ms, pST=pST: e.matmul(
                            pST, lhsT=kT[:, h, dc, ms], rhs=qT[:, h, dc, ms], start=(dc == 0), stop=(dc == 1)),
                            reads=k_kT(h, dc) + k_qT(h, dc), writes=psk(0))
                    P.add("dve", lambda e, par=par, pST=pST, w1c=w1c: e.scalar_tensor_tensor(
                        out=SpT[:, par, :], in0=pST, scalar=w1c, in1=cmask[:, :], op0=ALU.mult, op1=ALU.mult),
                        reads=psk(0) + ["cols", "cmask"], writes=[("SpT", par)])
                    for dc in range(2):
                        P.add("pe", lambda e, dc=dc, h=h, ms=ms, pKT=pKT: e.transpose(
                            out=pKT[:, dc * 128:(dc + 1) * 128], in_=kT[:, h, dc, ms], identity=idt[:]),
                            reads=k_kT(h, dc) + ["idt"], writes=psk(1))
                    P.add("act", lambda e, par=par, pKT=pKT, w1c=w1c: e.activation(
                        out=ktil[:, par, :], in_=pKT, func=AF.Copy, scale=w1c),
                        reads=psk(1) + ["cols"], writes=[("ktil", par)])
                if stage == 2:
                    P.add("pe", lambda e, par=par, m=m, h=h, pN=pN: e.matmul(
                        pN, lhsT=SpT[:, par, :], rhs=vext[:, m, h, 0:257], start=True, stop=False),
                        reads=[("SpT", par)] + k_v(m, h), writes=psk(3))
                    for dc in range(2):
                        P.add("pe", lambda e, dc=dc, h=h, ms=ms, pN=pN: e.matmul(
                            pN, lhsT=qT[:, h, dc, ms], rhs=Gbf[:, h, dc, 0:257], start=False, stop=(dc == 1)),
                            reads=k_qT(h, dc) + ["Gbf_%d" % h], writes=psk(3))
                    P.add("act", lambda e, s1=s1, pN=pN: e.activation(out=s1, in_=pN[:, 256:257], func=AF.Abs),
                          reads=psk(3), writes=kst)
                    P.add("dve", lambda e, s1=s1, enc=enc: e.tensor_tensor(out=s1, in0=s1, in1=enc, op=ALU.max),
                          reads=kst + ["cols"], writes=kst)
                    P.add("act", lambda e, ssn=ssn, pN=pN: e.activation(
                        out=junk_s[:, 0:256], in_=pN[:, 0:256], func=AF.Square, accum_out=ssn),
                        reads=psk(3), writes=["junk_s", ("ssn", par)])
                    P.add("dve", lambda e, s1=s1, s2=s2: e.tensor_tensor(out=s2, in0=s1, in1=s1, op=ALU.mult),
                          reads=kst, writes=kst)
                    P.add("dve", lambda e, s2=s2, ssn=ssn: e.scalar_tensor_tensor(
                        out=s2, in0=s2, scalar=256.0 * EPS, in1=ssn, op0=ALU.mult, op1=ALU.add),
                        reads=kst + [("ssn", par)], writes=kst)
                    P.add("act", lambda e, s2=s2: e.activation(out=s2, in_=s2, func=AF.Sqrt, scale=1.0 / 256.0),
                          reads=kst, writes=kst)
                    P.add("dve", lambda e, s2=s2: e.reciprocal(out=s2, in_=s2), reads=kst, writes=kst)
                    P.add("dve", lambda e, par=par, h=h, s2=s2, pN=pN: e.scalar_tensor_tensor(
                        out=hn[:, par, :], in0=pN[:, 0:256], scalar=s2, in1=ghead[:, h * 256:(h + 1) * 256],
                        op0=ALU.mult, op1=ALU.mult),
                        reads=psk(3) + kst + ["ghead"], writes=[("hn", par)])
                    for dc in range(2):
                        P.add("pe", lambda e, dc=dc, par=par, m=m, h=h, pP=pP: e.matmul(
                            pP[dc], lhsT=ktil[:, par, dc * 128:(dc + 1) * 128], rhs=vext[:, m, h, 0:257],
                            start=True, stop=True),
                            reads=[("ktil", par)] + k_v(m, h), writes=psk(4 + dc))
                    P.add("act", lambda e, par=par, h=h, dcc=dcc: e.activation(
                        out=Gs[:, par, :, :], in_=G32[:, h, :, :], func=AF.Copy, scale=dcc),
                        reads=["G32_%d" % h, "decbc"], writes=[("Gs", par)])
                    for dc in range(2):
                        P.add("dve", lambda e, dc=dc, par=par, h=h, dcc=dcc, pP=pP: e.scalar_tensor_tensor(
                            out=G32[:, h, dc, :], in0=pP[dc], scalar=dcc, in1=Gs[:, par, dc, :],
                            op0=ALU.mult, op1=ALU.add),
                            reads=psk(4 + dc) + [("Gs", par), "decbc"], writes=["G32_%d" % h])
                    P.add("act", lambda e, h=h: e.activation(out=Gbf[:, h, :, 0:257], in_=G32[:, h, :, :], func=AF.Copy),
                          reads=["G32_%d" % h], writes=["Gbf_%d" % h])
                if stage == 3:
                    for ec in range(2):
                        P.add("pe", lambda e, ec=ec, par=par, pHT=pHT: e.transpose(
                            out=pHT[:, ec * 128:(ec + 1) * 128], in_=hn[:, par, ec * 128:(ec + 1) * 128], identity=idt[:]),
                            reads=[("hn", par), "idt"], writes=psk(2))
                    P.add("dve", lambda e, h=h, ms=ms, pHT=pHT: e.tensor_tensor(
                        out=yT[:, 8 + 2 * h:10 + 2 * h, ms], in0=pHT.rearrange("p (c t) -> p c t", c=2),
                        in1=soT[:, h, :, ms], op=ALU.mult),
                        reads=psk(2) + k_soT(h, 0) + k_soT(h, 1), writes=[("R", 8 + 2 * h), ("R", 9 + 2 * h)])

            steps = [(m, h) for m in range(4) for h in range(4)]
            conv_calls = o_calls + conv_calls
            cci = 0
            for it in range(len(steps) + 2):
                if it < len(steps):
                    ml_step(steps[it][0], steps[it][1], 1)
                if 0 <= it - 1 < len(steps):
                    ml_step(steps[it - 1][0], steps[it - 1][1], 2)
                if 0 <= it - 2 < len(steps):
                    ml_step(steps[it - 2][0], steps[it - 2][1], 3)
                if cci < len(conv_calls):
                    conv_calls[cci]()
                    cci += 1
            while cci < len(conv_calls):
                conv_calls[cci]()
                cci += 1
            if dbg and b == 0:
                P.add("sp", lambda e: e.dma_start(out=d_yT, in_=yT[:, :, :].rearrange("p k t -> p (k t)")),
                      reads=[("R", k) for k in range(16)], dma="d_yT")

            for n in range(4):
                tm_proj(w_out, n * 512, yT, lambda k: [("R", k)], 16, post_evac(n, 24, 0), wname="w_out",
                        bank0=4 * (n % 2))
            rss = [post_rstd(m, 24) for m in range(4)]
            for m in range(4):
                P.add("dve", lambda e, m=m, rs=rss[m]: e.scalar_tensor_tensor(
                    out=xres[:, m, :], in0=y2s[:, m, :], scalar=rs, in1=xres[:, m, :], op0=ALU.mult, op1=ALU.add),
                    reads=k_y2all(m) + [("rsp", m), ("xres", m)], writes=[("xres", m)])
            if not (dbg and b == 0):
                for m in range(4):
                    norm_pre_sq(m)
                for m in range(4):
                    norm_pre_rs(m)
                for m in range(4):
                    norm_pre_scale(m)
            if dbg and b == 0:
                P.add("sp", lambda e: e.dma_start(out=d_x1, in_=xres[:, :, :].rearrange("p m d -> p (m d)")),
                      reads=[("xres", m) for m in range(4)], dma="d_x1")
            if dbg and b == 0:
                for m in range(4):
                    norm_pre(m)
            norm_tr(bufA, k_uT, 16, (0, 1))
            norm_tr(bufA, k_uT, 16, (2, 3))

            for fp in range(32):
                def ev_h(ci, bank, bkey, fp=fp):
                    f = 2 * fp + ci
                    P.add("act", lambda e: e.activation(out=rbuf[:, ci, :], in_=bank[:, :], func=AF.Relu),
                          reads=bkey, writes=[("rbuf", ci)])
                    P.add("dve", lambda e: e.tensor_tensor(out=hT[:, f, :], in0=rbuf[:, ci, :], in1=rbuf[:, ci, :],
                                                           op=ALU.mult),
                          reads=[("rbuf", ci)], writes=[("R", f)])
                fm_proj(w1, fp * 256, bufA, k_uT, ev_h, wname="w1")
                if b + 1 < nblk and fp >= 3 and (fp - 3) % 6 == 0 and (fp - 3) // 6 < 4:
                    stageA_part1(b + 1, (fp - 3) // 6)

            for n in range(4):
                tm_proj(w2, n * 512, hT, lambda k: [("R", k)], 64, post_evac(n, 40, 1), wname="w2", bank0=4 * (n % 2))
                if b + 1 < nblk and n == 1:
                    stageA_part2(0)
            rss = [post_rstd(m, 40) for m in range(4)]
            for m in range(4):
                P.add("dve", lambda e, m=m, rs=rss[m]: e.scalar_tensor_tensor(
                    out=y2s[:, m, :], in0=y2s[:, m, :], scalar=rs, in1=xres[:, m, :], op0=ALU.mult, op1=ALU.add),
                    reads=k_y2all(m) + [("rsp", m), ("xres", m)], writes=k_y2all(m))
                r0 = t0 + m * 128
                P.add(mq, lambda e, m=m, r0=r0: e.dma_start(out=out[r0:r0 + 128, :], in_=y2s[:, m, :]),
                      reads=k_y2all(m), dma=("xout", m))

        P.emit(st)
    return nc


def host_consts(inp):
    g_pre_mix = np.asarray(inp["g_pre_mix"], np.float32)[0]
    g_pre_mlp = np.asarray(inp["g_pre_mlp"], np.float32)[0]
    gcol = np.concatenate([g_pre_mix.reshape(16, 128).T, g_pre_mlp.reshape(16, 128).T], axis=1)
    gpost = np.stack([np.broadcast_to(np.asarray(inp["g_post_mix"], np.float32)[0], (128, D)),
                      np.broadcast_to(np.asarray(inp["g_post_mlp"], np.float32)[0], (128, D))], axis=1)
    ghead = np.broadcast_to(np.asarray(inp["g_head"], np.float32)[0], (128, 1024))
    convw = np.asarray(inp["conv_w"], np.float32)[0].reshape(3, 8, 128).transpose(2, 1, 0)
    bif = np.stack([np.asarray(inp["b_i"], np.float32)[0], np.asarray(inp["b_f"], np.float32)[0]], axis=1)
    ident = np.eye(128, dtype=np.float32).astype(ml_dtypes.bfloat16)
    cmask = np.triu(np.ones((128, 128), np.float32))
    sel = np.zeros((4, 512), np.float32)
    for h in range(4):
        sel[h, h * 128:(h + 1) * 128] = 1.0
    i4 = np.eye(4, dtype=np.float32)
    c = dict(gcol=gcol, gpost=gpost, ghead=ghead, convw=convw, bif=bif, ident=ident, cmask=cmask, sel=sel, i4=i4)
    return {k: np.ascontiguousarray(v) for k, v in c.items()}


def kernel(**inputs):
    n = 8
    x = np.asarray(inputs["x"], np.float32)
    shared = host_consts(inputs)
    shared["w_in"] = np.ascontiguousarray(np.asarray(inputs["w_in"], np.float32)[0])
    shared["w_out"] = np.ascontiguousarray(np.asarray(inputs["w_out"], np.float32)[0])
    shared["w_mlp1"] = np.ascontiguousarray(np.asarray(inputs["w_mlp1"], np.float32)[0])
    shared["w_mlp2"] = np.ascontiguousarray(np.asarray(inputs["w_mlp2"], np.float32)[0])
    nc = build_nc()
    in_maps = []
    for c in range(n):
        d = dict(shared)
        d["x"] = np.ascontiguousarray(x[c])
        in_maps.append(d)
    res = run_bass_kernel_spmd(nc, in_maps, core_ids=list(range(n)))
    return np.stack([np.asarray(r["out"], np.float32) for r in res.results], axis=0)
```

```python
import math
from contextlib import ExitStack

import numpy as np
import ml_dtypes
import concourse.bass as bass
import concourse.mybir as mybir
from concourse.bass_utils import run_bass_kernel_spmd

F32 = mybir.dt.float32
BF16 = mybir.dt.bfloat16
AF = mybir.ActivationFunctionType
ALU = mybir.AluOpType
AX = mybir.AxisListType

S = 2048
D = 2048
T = 512
NB = 4
DI = 7176
DFF = 8192
EPS = 1e-6
NSLOT = 4
USE_SCRATCH = False
ENGS = ("pe", "act", "dve", "pool", "sp")


class Op:
    __slots__ = ("eng", "fn", "deps", "sig", "sem", "val", "dma", "name")

    def __init__(self, eng, fn, dma, name):
        self.eng = eng
        self.fn = fn
        self.deps = []
        self.sig = False
        self.sem = None
        self.val = 0
        self.dma = dma
        self.name = name


class Prog:
    def __init__(self, nc):
        self.nc = nc
        self.ops = {e: [] for e in ENGS}
        self.last_w = {}
        self.readers = {}
        self.dma_sems = {}
        self.eng_sem = {}
        self.all = []
        self.bank_acc = {}

    def add(self, eng, fn, reads=(), writes=(), dma=None, name=""):
        op = Op(eng, fn, dma, name)
        deps = []
        banks = set(k[1] for k in list(reads) + list(writes) if isinstance(k, tuple) and k[0] == "ps")
        reads = [k for k in reads if not (isinstance(k, tuple) and k[0] == "ps")]
        writes = [k for k in writes if not (isinstance(k, tuple) and k[0] == "ps")]
        for bnk in sorted(banks):
            acc = self.bank_acc.setdefault(bnk, {})
            for e2, o2 in acc.items():
                if e2 != eng:
                    deps.append((o2, "bank"))
            acc[eng] = op
        for k in reads:
            w = self.last_w.get(k)
            if w is not None:
                deps.append((w, "raw"))
        for k in writes:
            w = self.last_w.get(k)
            if w is not None:
                deps.append((w, "waw"))
            for r in self.readers.get(k, ()):
                deps.append((r, "war"))
        seen = set()
        for (d, kind) in deps:
            if d is op:
                continue
            if d.eng == eng and d.dma is None and op.dma is None and eng == "pe":
                continue
            if id(d) in seen:
                continue
            seen.add(id(d))
            op.deps.append(d)
        for k in reads:
            self.readers.setdefault(k, []).append(op)
        for k in writes:
            self.last_w[k] = op
            self.readers[k] = []
        self.ops[eng].append(op)
        self.all.append(op)
        return op

    def emit(self, stack):
        nc = self.nc
        for e in ENGS:
            self.eng_sem[e] = stack.enter_context(nc.semaphore("es_" + e))
        nk = 0
        for op in self.all:
            if op.dma is not None and op.dma not in self.dma_sems:
                self.dma_sems[op.dma] = [stack.enter_context(nc.semaphore("ds_%d" % nk)), 0]
                nk += 1
        for op in self.all:
            for d in op.deps:
                d.sig = True
        cnt = {e: 0 for e in ENGS}
        for op in self.all:
            if op.dma is not None:
                s = self.dma_sems[op.dma]
                s[1] += 16
                op.sem = s[0]
                op.val = s[1]
                op.sig = True
            elif op.sig:
                cnt[op.eng] += 1
                op.sem = self.eng_sem[op.eng]
                op.val = cnt[op.eng]
        self.max_vals = dict(cnt)
        block = stack.enter_context(nc.Block())

        def run(engname, e):
            waited = {}
            for op in self.ops[engname]:
                need = {}
                for d in op.deps:
                    key = d.sem.num
                    if waited.get(key, 0) >= d.val:
                        continue
                    if key not in need or need[key][1] < d.val:
                        need[key] = (d.sem, d.val)
                for key, (sem, val) in need.items():
                    e.wait_ge(sem, val)
                    waited[key] = val
                ins = op.fn(e)
                if op.sig:
                    assert ins is not None, op.name
                    ins.then_inc(op.sem, 16 if op.dma is not None else 1)
            if engname == "sp":
                for k, (sem, val) in self.dma_sems.items():
                    if waited.get(sem.num, 0) < val:
                        e.wait_ge(sem, val)

        @block.sync
        def _(e):
            run("sp", e)

        @block.tensor
        def _(e):
            run("pe", e)

        @block.scalar
        def _(e):
            run("act", e)

        @block.vector
        def _(e):
            run("dve", e)

        @block.gpsimd
        def _(e):
            run("pool", e)


def pg(off, n):
    return [("R", p) for p in range(off // 1024, (off + n - 1) // 1024 + 1)]


def psk(b, q=None):
    if q is None:
        return [("ps", b, i) for i in range(4)]
    return [("ps", b, q)]


def build_nc(nblk=NB, dbg=False):
    nc = bass.Bass("TRN2", target_bir_lowering=False)

    def din(name, shape, dt=F32):
        return nc.dram_tensor(name, shape, dt, kind="ExternalInput").ap()

    def dout(name, shape, dt=F32):
        return nc.dram_tensor(name, shape, dt, kind="ExternalOutput").ap()

    x = din("x", [S, D])
    w_in = din("w_in", [D, DI])
    w_out = din("w_out", [D, D])
    w1 = din("w_mlp1", [D, DFF])
    w2 = din("w_mlp2", [DFF, D])
    gcol_d = din("gcol", [128, 32])
    gpost_d = din("gpost", [128, 2, D])
    ghead_d = din("ghead", [128, 1024])
    convw_d = din("convw", [128, 8, 3])
    bif_d = din("bif", [4, 2])
    ident_d = din("ident", [128, 128], BF16)
    cmask_d = din("cmask", [128, 128])
    sel_d = din("sel", [4, 512])
    i4_d = din("i4", [4, 4])
    out = dout("out", [S, D])
    NUNIT = 200 if USE_SCRATCH else 1
    scratch = nc.dram_tensor("wscr", [NUNIT, 128, 2048], BF16, kind="Internal").ap()
    if dbg:
        d_uT = dout("d_uT", [128, 16 * 512], BF16)
        d_gates = dout("d_gates", [4, 4 * 512])
        d_cols = dout("d_cols", [128, 32 + 16])
        d_yT = dout("d_yT", [128, 16 * 512], BF16)
        d_x1 = dout("d_x1", [128, 4 * D])
        d_qk = dout("d_qk", [128, 3 * 4 * 2 * 512], BF16)
        d_v = dout("d_v", [128, 4 * 4 * 258], BF16)

    with ExitStack() as st:
        P = Prog(nc)

        def sb(name, shape, dt=F32):
            return st.enter_context(nc.sbuf_tensor("s_" + name, shape, dt))

        xres = sb("xres", [128, 4, D])
        bufA = sb("bufA", [128, 16, T], BF16)
        R = sb("R", [128, 16384])
        y2s = sb("y2s", [128, 4, D])
        ring = sb("ring", [128, NSLOT, 2048], BF16)
        gpost = sb("gpost", [128, 2, D])
        ghead = sb("ghead", [128, 1024])
        G32 = sb("G32", [128, 4, 2, 257])
        Gbf = sb("Gbf", [128, 4, 2, 258], BF16)
        Gs = sb("Gs", [128, 2, 2, 257])
        halo = sb("halo", [128, 8, 2])
        convw = sb("convw", [128, 8, 3])
        gcol = sb("gcol", [128, 32])
        Wg = sb("Wg", [128, 16, 8], BF16)
        idt = sb("idt", [128, 128], BF16)
        cmask = sb("cmask", [128, 128])
        selt = sb("selt", [4, 512])
        i4t = sb("i4t", [4, 4])
        bif = sb("bif", [4, 2])
        negbf = sb("negbf", [4, 1])
        zer4 = sb("zer4", [4, 512])
        m_in = sb("m_in", [4, 1])
        mup = sb("mup", [4, 4])
        dec = sb("dec", [4, 4])
        SpT = sb("SpT", [128, 2, 128], BF16)
        ktil = sb("ktil", [128, 2, 256], BF16)
        hn = sb("hn", [128, 2, 256], BF16)
        rbuf = sb("rbuf", [128, 2, T], BF16)
        junk_s = sb("junk_s", [128, T], BF16)
        cols = sb("cols", [128, 4, 8])
        decbc = sb("decbc", [128, 16])
        stt = sb("stt", [128, 64])
        ps = [st.enter_context(nc.psum_tensor("ps%d" % i, [128, 512], F32)) for i in range(8)]

        hT = R[:, :].bitcast(BF16).rearrange("p (f t) -> p f t", f=64)
        yT = R[:, 0:4096].bitcast(BF16).rearrange("p (k t) -> p k t", k=16)
        uT = bufA
        qT = R[:, 4096:6144].bitcast(BF16).rearrange("p (h c t) -> p h c t", h=4, c=2)
        kT = R[:, 6144:8192].bitcast(BF16).rearrange("p (h c t) -> p h c t", h=4, c=2)
        soT = R[:, 8192:10240].bitcast(BF16).rearrange("p (h c t) -> p h c t", h=4, c=2)
        vext = R[:, 10240:12304].bitcast(BF16).rearrange("p (m h e) -> p m h e", m=4, h=4)
        VOFF = 40960
        UCOFF = 50176
        ucb = [R[:, 12544 + i * 514: 12544 + (i + 1) * 514] for i in range(2)]
        IBOFF = 55296
        interbc = R[:, 13824:15872].rearrange("p (h t) -> p h t", h=4)

        def k_uT(k):
            return [("A", k)]

        def k_qT(h, c):
            return [("R", 16 + 2 * h + c)]

        def k_kT(h, c):
            return [("R", 24 + 2 * h + c)]

        def k_soT(h, c):
            return [("R", 32 + 2 * h + c)]

        def k_v(m, h):
            return pg(VOFF + (m * 4 + h) * 516, 516)

        def k_uc(i):
            return pg(UCOFF + i * 2056, 2056)

        def k_ib(h):
            return pg(IBOFF + h * 2048, 2048)

        def ubf(m):
            return y2s[:, m, 0:1024].bitcast(BF16)

        def junkA(m):
            return y2s[:, m, 1024:2048].bitcast(BF16)

        def k_y2(m, n):
            if n < 2:
                return [("y2lo", m)]
            return [("y2hi", m, n - 2)]

        def k_y2all(m):
            return [("y2lo", m), ("y2hi", m, 0), ("y2hi", m, 1)]

        cx_s = [y2s[:, i, 1024:1536] for i in range(2)]
        ycv = [y2s[:, i, 1536:2048] for i in range(2)]
        GA = y2s[0:4, 2, 1024:1536]
        GB = y2s[0:4, 2, 1536:2048]
        GC = y2s[0:4, 3, 1024:1536]
        GD = y2s[0:4, 3, 1536:2048]
        kGA, kGB, kGC, kGD = [("y2hi", 2, 0)], [("y2hi", 2, 1)], [("y2hi", 3, 0)], [("y2hi", 3, 1)]

        ring_ctr = [0]

        def ring_next():
            s = ring_ctr[0] % NSLOT
            ring_ctr[0] += 1
            return s

        def ring_fm(s):
            return ring[:, s, :].rearrange("p (kk c) -> p kk c", kk=8)

        def ring_tm(s):
            return ring[:, s, :].rearrange("p (kk c) -> p kk c", kk=4)

        def ld(dst, src, key):
            P.add("sp", lambda e: e.dma_start(out=dst, in_=src), writes=[key], dma=key)

        ld(idt[:], ident_d, "idt")
        ld(gcol[:], gcol_d, "gcol")
        ld(bif[:], bif_d, "bif")
        late_loads = [(gpost[:], gpost_d, "gpost"), (ghead[:], ghead_d, "ghead"), (convw[:], convw_d, "convw"),
                      (cmask[:], cmask_d, "cmask"), (selt[:], sel_d, "selt"), (i4t[:], i4_d, "i4t")]
        P.add("pool", lambda e: e.dma_start(
            out=Wg[:], in_=w_in[:, 7168:7176].rearrange("(k p) c -> p k c", p=128)), writes=["Wg"], dma="Wg")
        P.add("dve", lambda e: e.memset(G32[:], 0.0), writes=["G32_%d" % h for h in range(4)])
        P.add("dve", lambda e: e.memset(Gbf[:], 0.0), writes=["Gbf_%d" % h for h in range(4)])
        P.add("dve", lambda e: e.memset(halo[:], 0.0), writes=["halo"])
        P.add("dve", lambda e: e.memset(zer4[:], 0.0), writes=["zer4"])
        P.add("dve", lambda e: e.memset(m_in[:], 0.0), writes=["m_in"])
        P.add("dve", lambda e: e.tensor_scalar(out=negbf[:], in0=bif[:, 1:2], scalar1=-1.0, scalar2=None, op0=ALU.mult),
              reads=["bif"], writes=["negbf"])

        def rstd_ops(ss_ap, rs_ap, n, kss, krs):
            P.add("act", lambda e: e.activation(out=rs_ap, in_=ss_ap, func=AF.Sqrt, scale=1.0 / n, bias=EPS),
                  reads=kss, writes=krs)
            P.add("dve", lambda e: e.reciprocal(out=rs_ap, in_=rs_ap), reads=krs, writes=krs)

        def norm_pre_sq(m):
            ss = stt[:, m:m + 1]
            P.add("act", lambda e: e.activation(out=junkA(m), in_=xres[:, m, :], func=AF.Square, accum_out=ss),
                  reads=[("xres", m)], writes=[("y2hi", m, 0), ("y2hi", m, 1), ("ss", m)])

        def norm_pre_rs(m):
            rstd_ops(stt[:, m:m + 1], stt[:, 4 + m:5 + m], D, [("ss", m)], [("rs", m)])

        def norm_pre_scale(m):
            rs = stt[:, 4 + m:5 + m]
            if m % 2 == 0:
                P.add("dve", lambda e: e.tensor_scalar(out=ubf(m), in0=xres[:, m, :], scalar1=rs, scalar2=None,
                                                       op0=ALU.mult),
                      reads=[("xres", m), ("rs", m)], writes=[("y2lo", m)])
            else:
                P.add("act", lambda e: e.activation(out=ubf(m), in_=xres[:, m, :], func=AF.Copy, scale=rs),
                      reads=[("xres", m), ("rs", m)], writes=[("y2lo", m)])

        def norm_pre(m):
            norm_pre_sq(m)
            norm_pre_rs(m)
            norm_pre_scale(m)

        def norm_tr(dstT, dkey, goff, mlist, srcf=None, skeyf=None, bank0=0):
            if srcf is None:
                srcf = ubf
                skeyf = lambda m: [("y2lo", m)]
            m0 = mlist[0]
            nm = len(mlist)
            for k in range(16):
                bb = bank0 + k % 4
                pT = ps[bb][:, 0:256].bitcast(BF16)
                for j, m in enumerate(mlist):
                    P.add("pe", lambda e, k=k, m=m, j=j, pT=pT: e.transpose(
                        out=pT[:, j * 128:(j + 1) * 128], in_=srcf(m)[:, k * 128:(k + 1) * 128], identity=idt[:]),
                        reads=skeyf(m) + ["idt"], writes=psk(bb))
                gc = gcol[:, goff + k:goff + k + 1]
                dst = dstT[:, k, m0 * 128:(m0 + nm) * 128]
                src = pT[:, 0:nm * 128]
                if k % 2 == 0:
                    P.add("dve", lambda e, dst=dst, src=src, gc=gc: e.tensor_scalar(
                        out=dst, in0=src, scalar1=gc, scalar2=None, op0=ALU.mult),
                        reads=psk(bb) + ["gcol"], writes=dkey(k))
                else:
                    P.add("act", lambda e, dst=dst, src=src, gc=gc: e.activation(
                        out=dst, in_=src, func=AF.Copy, scale=gc),
                        reads=psk(bb) + ["gcol"], writes=dkey(k))

        def norm_T(dstT, dkey, goff, tag):
            for m in range(4):
                norm_pre(m)
            norm_tr(dstT, dkey, goff, (0, 1, 2, 3))

        def ubfN(m):
            return y2s[:, m, 1024:2048].bitcast(BF16)

        def k_ubfN(m):
            return [("y2hi", m, 0), ("y2hi", m, 1)]

        def stageA_part1(bn, m):
            i = m % 2
            r0 = bn * T + m * 128
            L = y2s[:, 2 * i:2 * i + 2, 0:1024]
            kL = [("y2lo", 2 * i), ("y2lo", 2 * i + 1)]
            u3 = ubfN(m).rearrange("p (a c) -> p a c", a=2)
            ss = stt[:, 56 + m:57 + m]
            rs = stt[:, 60 + m:61 + m]
            P.add("sp", lambda e: e.dma_start(out=L, in_=x[r0:r0 + 128, :].rearrange("p (a c) -> p a c", a=2)),
                  writes=kL, dma=("xpre", i))
            P.add("act", lambda e: e.activation(out=u3, in_=L, func=AF.Square, accum_out=ss),
                  reads=kL, writes=k_ubfN(m) + [("ssN", m)])
            rstd_ops(ss, rs, D, [("ssN", m)], [("rsN", m)])
            P.add("dve", lambda e: e.tensor_scalar(out=u3, in0=L, scalar1=rs, scalar2=None, op0=ALU.mult),
                  reads=kL + [("rsN", m)], writes=k_ubfN(m))

        def stageA_part2(bank0):
            norm_tr(bufA, k_uT, 0, (0, 1, 2, 3), srcf=ubfN, skeyf=k_ubfN, bank0=bank0)

        fm_ctr = [0]
        scr_idx = {}
        scr_ctr = [0]
        n_new = [0]
        cur_b = [0]

        def load_unit(s, wname, W, kind, r0, c0):
            key = (wname, kind, r0, c0)
            if key in scr_idx:
                idx = scr_idx[key]
                P.add("sp", lambda e: e.dma_start(out=ring[:, s, :], in_=scratch[idx]),
                      reads=[("scr", idx)], writes=[("ring", s)], dma=("ring", s))
                return
            if kind == "fm":
                dst = ring_fm(s)
                src = W[r0:r0 + 1024, c0:c0 + 256].rearrange("(kk p) c -> p kk c", p=128)
            else:
                dst = ring_tm(s)
                src = W[r0:r0 + 512, c0:c0 + 512].rearrange("(kk p) c -> p kk c", p=128)
            P.add("pool", lambda e: e.dma_start(out=dst, in_=src), writes=[("ring", s)], dma=("ring", s))
            n_new[0] += 1
            if USE_SCRATCH and (cur_b[0] >= 1 or n_new[0] % 2 == 0):
                idx = scr_ctr[0]
                scr_ctr[0] += 1
                assert idx < NUNIT
                scr_idx[key] = idx
                P.add("sp", lambda e: e.dma_start(out=scratch[idx], in_=ring[:, s, :]),
                      reads=[("ring", s)], writes=[("scr", idx)], dma=("scrst", s))

        def fm_proj(W, col0, src, skey, evac, banks=None, wname="w_in"):
            if banks is None:
                par = fm_ctr[0] % 2
                fm_ctr[0] += 1
                banks = (4 + 2 * par, 5 + 2 * par)
            for half in range(2):
                s = ring_next()
                load_unit(s, wname, W, "fm", half * 1024, col0)
                for kk in range(8):
                    k = half * 8 + kk
                    for ci in range(2):
                        P.add("pe", lambda e, s=s, kk=kk, k=k, ci=ci: e.matmul(
                            ps[banks[ci]][:, :], lhsT=ring_fm(s)[:, kk, ci * 128:(ci + 1) * 128], rhs=src[:, k, :],
                            start=(k == 0), stop=(k == 15)),
                            reads=[("ring", s)] + skey(k), writes=psk(banks[ci]))
            for ci in range(2):
                evac(ci, ps[banks[ci]], psk(banks[ci]))

        def tm_proj(W, col0, src, skey, nkt, evac, mlist=(0, 1, 2, 3), wname="w_in", bank0=0):
            for u in range(nkt // 4):
                s = ring_next()
                load_unit(s, wname, W, "tm", u * 512, col0)
                for kk in range(4):
                    k = u * 4 + kk
                    for m in mlist:
                        P.add("pe", lambda e, s=s, kk=kk, k=k, m=m: e.matmul(
                            ps[bank0 + m][:, :], lhsT=src[:, k, m * 128:(m + 1) * 128], rhs=ring_tm(s)[:, kk, :],
                            start=(k == 0), stop=(k == nkt - 1)),
                            reads=[("ring", s)] + skey(k), writes=psk(bank0 + m))
            for m in mlist:
                evac(m, ps[bank0 + m], psk(bank0 + m))

        def post_evac(n, ssq_off, gi):
            def ev(m, bank, bkey):
                dst = y2s[:, m, n * 512:(n + 1) * 512]
                sq = stt[:, ssq_off + m * 4 + n: ssq_off + m * 4 + n + 1]
                P.add("act", lambda e: e.activation(out=junk_s[:], in_=bank[:, :], func=AF.Square, accum_out=sq),
                      reads=bkey, writes=["junk_s", ("ssq", m, n)])
                P.add("dve", lambda e: e.tensor_tensor(out=dst, in0=bank[:, :], in1=gpost[:, gi, n * 512:(n + 1) * 512],
                                                       op=ALU.mult),
                      reads=bkey + ["gpost"], writes=k_y2(m, n))
            return ev

        def post_rstd(m, ssq_off):
            ssv = stt[:, ssq_off + m * 4: ssq_off + m * 4 + 4]
            ss = stt[:, 8 + m:9 + m]
            rs = stt[:, 12 + m:13 + m]
            P.add("dve", lambda e: e.reduce_sum(out=ss, in_=ssv, axis=AX.X),
                  reads=[("ssq", m, n) for n in range(4)], writes=[("ssp", m)])
            rstd_ops(ss, rs, D, [("ssp", m)], [("rsp", m)])
            return rs

        for b in range(nblk):
            t0 = b * T
            cur_b[0] = b
            mq = "sp" if (b == 0 or not USE_SCRATCH) else "pool"
            if b == 0:
                for m in range(4):
                    stageA_part1(0, m)
                for (dst_, src_, key_) in late_loads:
                    ld(dst_, src_, key_)
                stageA_part2(0)
            for m in range(4):
                r0 = t0 + m * 128
                P.add(mq, lambda e, m=m, r0=r0: e.dma_start(out=xres[:, m, :], in_=x[r0:r0 + 128, :]),
                      writes=[("xres", m)], dma=("xin", m))
            if dbg and b == 0:
                P.add("sp", lambda e: e.dma_start(out=d_uT, in_=bufA[:, :, :].rearrange("p k t -> p (k t)")),
                      reads=[("A", k) for k in range(16)], dma="d_uT")

            psi = ps[2][0:4, :]
            psf = ps[3][0:4, :]
            for gi, pso, bk in ((0, psi, 2), (1, psf, 3)):
                for k in range(16):
                    P.add("pe", lambda e, k=k, gi=gi, pso=pso: e.matmul(
                        pso, lhsT=Wg[:, k, gi * 4:(gi + 1) * 4], rhs=uT[:, k, :], start=(k == 0), stop=(k == 15)),
                        reads=k_uT(k) + ["Wg"], writes=psk(bk))
            P.add("act", lambda e: e.activation(out=GA, in_=psf, func=AF.Exp, scale=-1.0, bias=negbf[:, 0:1]),
                  reads=psk(3) + ["negbf"], writes=kGA)
            P.add("act", lambda e: e.activation(out=GA, in_=GA, func=AF.Ln, scale=1.0, bias=1.0),
                  reads=kGA, writes=kGA)
            P.add("dve", lambda e: e.tensor_tensor_scan(out=GB, data0=GA, data1=zer4[:], initial=0.0,
                                                        op0=ALU.add, op1=ALU.add),
                  reads=kGA + ["zer4"], writes=kGB)
            P.add("dve", lambda e: e.scalar_tensor_tensor(out=GA, in0=psi, scalar=bif[:, 0:1], in1=GB,
                                                          op0=ALU.add, op1=ALU.add),
                  reads=psk(2) + kGB + ["bif"], writes=kGA)
            P.add("dve", lambda e: e.tensor_tensor_scan(out=GC, data0=GA, data1=GA, initial=m_in[:, 0:1],
                                                        op0=ALU.max, op1=ALU.max),
                  reads=kGA + ["m_in"], writes=kGC)
            P.add("dve", lambda e: e.tensor_copy(out=mup[:, 0:1], in_=m_in[:, 0:1]), reads=["m_in"], writes=["mup0"])
            P.add("dve", lambda e: e.tensor_copy(out=mup[:, 1:4], in_=GC[:, 127:384:128]), reads=kGC, writes=["mup1"])
            P.add("dve", lambda e: e.tensor_tensor(out=m_in[:, 0:1], in0=GC[:, 511:512], in1=GB[:, 511:512],
                                                   op=ALU.subtract),
                  reads=kGC + kGB, writes=["m_in"])
            mup_bc = mup[:, :].rearrange("p (c o) -> p c o", o=1).to_broadcast([4, 4, 128])

            def v3(ap):
                return ap.rearrange("p (c t) -> p c t", c=4)
            P.add("dve", lambda e: e.tensor_tensor(out=v3(GD), in0=mup_bc, in1=v3(GC), op=ALU.subtract),
                  reads=["mup0", "mup1"] + kGC, writes=kGD)
            P.add("act", lambda e: e.activation(out=GD, in_=GD, func=AF.Exp, scale=1.0, bias=math.log(1.0 / 16.0)),
                  reads=kGD, writes=kGD)
            P.add("dve", lambda e: e.tensor_tensor(out=dec[:, :], in0=mup[:, :], in1=GC[:, 127:512:128],
                                                   op=ALU.subtract),
                  reads=["mup0", "mup1"] + kGC, writes=["dec"])
            P.add("act", lambda e: e.activation(out=dec[:, :], in_=dec[:, :], func=AF.Exp), reads=["dec"], writes=["dec"])
            P.add("dve", lambda e: e.tensor_tensor(out=GB, in0=GB, in1=GC, op=ALU.subtract),
                  reads=kGB + kGC, writes=kGB)
            P.add("act", lambda e: e.activation(out=GB, in_=GB, func=AF.Exp), reads=kGB, writes=kGB)
            P.add("dve", lambda e: e.tensor_tensor(out=v3(GA), in0=v3(GA), in1=mup_bc, op=ALU.subtract),
                  reads=["mup0", "mup1"] + kGA, writes=kGA)
            P.add("act", lambda e: e.activation(out=GA, in_=GA, func=AF.Exp), reads=kGA, writes=kGA)
            if dbg and b == 0:
                for i, (g, kk) in enumerate(((GA, kGA), (GB, kGB), (GC, kGC), (GD, kGD))):
                    P.add("sp", lambda e, i=i, g=g: e.dma_start(out=d_gates[:, i * 512:(i + 1) * 512], in_=g),
                          reads=kk, dma=("d_g", i))
            conv_calls = []
            for cp in range(4):
                def ev_x(ci, bank, bkey):
                    P.add("act", lambda e: e.activation(out=cx_s[ci], in_=bank[:, :], func=AF.Copy),
                          reads=bkey, writes=[("y2hi", ci, 0)])

                def ev_c(ci, bank, bkey, cp=cp):
                    cb = cp * 2 + ci
                    uc = ucb[ci]
                    P.add("dve", lambda e: e.tensor_copy(out=uc[:, 0:2], in_=halo[:, cb, :]),
                          reads=["halo"], writes=k_uc(ci))
                    P.add("dve", lambda e: e.tensor_tensor(out=uc[:, 2:514], in0=bank[:, :], in1=cx_s[ci], op=ALU.mult),
                          reads=bkey + [("y2hi", ci, 0)], writes=k_uc(ci))
                    P.add("dve", lambda e: e.tensor_copy(out=halo[:, cb, :], in_=uc[:, 512:514]),
                          reads=k_uc(ci), writes=["halo"])
                    P.add("act", lambda e: e.activation(out=ycv[ci], in_=uc[:, 0:512], func=AF.Copy,
                                                        scale=convw[:, cb, 0:1]),
                          reads=k_uc(ci) + ["convw"], writes=[("y2hi", ci, 1)])
                    for tap in (1, 2):
                        P.add("dve", lambda e, tap=tap: e.scalar_tensor_tensor(
                            out=ycv[ci], in0=uc[:, tap:tap + 512], scalar=convw[:, cb, tap:tap + 1], in1=ycv[ci],
                            op0=ALU.mult, op1=ALU.add),
                            reads=k_uc(ci) + ["convw", ("y2hi", ci, 1)], writes=[("y2hi", ci, 1)])

                def ev_b(ci, bank, bkey, cp=cp):
                    cb = cp * 2 + ci
                    P.add("dve", lambda e: e.tensor_tensor(out=yT[:, cb, :], in0=bank[:, :], in1=ycv[ci], op=ALU.mult),
                          reads=bkey + [("y2hi", ci, 1)], writes=[("R", cb)])
                conv_calls.append(lambda cp=cp, ev=ev_x: fm_proj(w_in, cp * 256, uT, k_uT, ev, banks=(6, 7)))
                conv_calls.append(lambda cp=cp, ev=ev_c: fm_proj(w_in, 2048 + cp * 256, uT, k_uT, ev, banks=(6, 7)))
                conv_calls.append(lambda cp=cp, ev=ev_b: fm_proj(w_in, 1024 + cp * 256, uT, k_uT, ev, banks=(6, 7)))

            P.add("dve", lambda e: e.memset(vext[:, :, :, 256:258], 1.0), writes=pg(VOFF, 8256))
            for half in range(2):
                def ev_v(m, bank, bkey, half=half):
                    dst = vext[:, m, 2 * half:2 * half + 2, 0:256]
                    src = bank[:, :].rearrange("p (h e) -> p h e", h=2)
                    kk = k_v(m, 2 * half) + k_v(m, 2 * half + 1)
                    if m % 2 == 0:
                        P.add("dve", lambda e: e.tensor_copy(out=dst, in_=src), reads=bkey, writes=kk)
                    else:
                        P.add("act", lambda e: e.activation(out=dst, in_=src, func=AF.Copy), reads=bkey, writes=kk)
                tm_proj(w_in, 5120 + half * 512, uT, k_uT, 16, ev_v, bank0=4 * half)
            o_calls = []
            q_calls = []
            for h in range(4):
                def ev_q(ci, bank, bkey, h=h):
                    P.add("dve", lambda e: e.tensor_tensor(out=qT[:, h, ci, :], in0=bank[:, :], in1=interbc[:, h, :],
                                                           op=ALU.mult),
                          reads=bkey + k_ib(h), writes=k_qT(h, ci))

                def ev_k(ci, bank, bkey, h=h):
                    P.add("act", lambda e: e.activation(out=kT[:, h, ci, :], in_=bank[:, :], func=AF.Copy),
                          reads=bkey, writes=k_kT(h, ci))

                def ev_o(ci, bank, bkey, h=h):
                    P.add("act", lambda e: e.activation(out=soT[:, h, ci, :], in_=bank[:, :], func=AF.Sigmoid),
                          reads=bkey, writes=k_soT(h, ci))
                q_calls.append(lambda h=h, ev=ev_q: fm_proj(w_in, 3072 + h * 256, uT, k_uT, ev))
                fm_proj(w_in, 4096 + h * 256, uT, k_uT, ev_k)
                o_calls.append(lambda h=h, ev=ev_o: fm_proj(w_in, 6144 + h * 256, uT, k_uT, ev, banks=(6, 7)))
            for h in range(4):
                P.add("pe", lambda e, h=h: e.matmul(ps[4 + h][:, :], lhsT=selt[:, h * 128:(h + 1) * 128], rhs=GD,
                                                    start=True, stop=True),
                      reads=["selt"] + kGD, writes=psk(4 + h))
                if h % 2 == 0:
                    P.add("dve", lambda e, h=h: e.tensor_copy(out=interbc[:, h, :], in_=ps[4 + h][:, :]),
                          reads=psk(4 + h), writes=k_ib(h))
                else:
                    P.add("act", lambda e, h=h: e.activation(out=interbc[:, h, :], in_=ps[4 + h][:, :], func=AF.Copy),
                          reads=psk(4 + h), writes=k_ib(h))
            for m in range(4):
                P.add("pe", lambda e, m=m: e.matmul(ps[2][:, m * 8:m * 8 + 4], lhsT=GA[:, m * 128:(m + 1) * 128],
                                                    rhs=i4t[:, :], start=True, stop=True),
                      reads=kGA + ["i4t"], writes=psk(2))
                P.add("pe", lambda e, m=m: e.matmul(ps[2][:, m * 8 + 4:m * 8 + 8], lhsT=GB[:, m * 128:(m + 1) * 128],
                                                    rhs=i4t[:, :], start=True, stop=True),
                      reads=kGB + ["i4t"], writes=psk(2))
            P.add("dve", lambda e: e.tensor_copy(out=cols[:, :, :], in_=ps[2][:, 0:32].rearrange("p (m c) -> p m c", m=4)),
                  reads=psk(2), writes=["cols"])
            for h in range(4):
                P.add("pe", lambda e, h=h: e.matmul(ps[3][:, h * 4:(h + 1) * 4], lhsT=selt[:, h * 128:(h + 1) * 128],
                                                    rhs=dec[:, :], start=True, stop=True),
                      reads=["selt", "dec"], writes=psk(3))
            P.add("dve", lambda e: e.tensor_copy(out=decbc[:, :], in_=ps[3][:, 0:16]), reads=psk(3), writes=["decbc"])
            if dbg and b == 0:
                P.add("sp", lambda e: e.dma_start(out=d_cols[:, 0:32], in_=cols[:, :, :].rearrange("p m c -> p (m c)")),
                      reads=["cols"], dma="d_c1")
                P.add("sp", lambda e: e.dma_start(out=d_cols[:, 32:48], in_=decbc[:, :]), reads=["decbc"], dma="d_c2")

            for qc in q_calls:
                qc()
            if dbg and b == 0:
                P.add("sp", lambda e: e.dma_start(out=d_qk, in_=R[:, 4096:10240].bitcast(BF16)),
                      reads=[("R", p) for p in range(16, 40)], dma="d_qk")
                P.add("sp", lambda e: e.dma_start(out=d_v, in_=R[:, 10240:12304].bitcast(BF16)),
                      reads=pg(VOFF, 8256), dma="d_v")

            def ml_step(m, h, stage):
                ms = slice(m * 128, (m + 1) * 128)
                par = h % 2
                pST = ps[0][:, 0:128]
                pKT = ps[1][:, 0:128].bitcast(BF16)
                pHT = ps[2][:, 0:128].bitcast(BF16)
                pN = ps[3][:, 0:257]
                pP = [ps[4][:, 0:257], ps[5][:, 0:257]]
                w1c = cols[:, m, h:h + 1]
                enc = cols[:, m, 4 + h:5 + h]
                dcc = decbc[:, h * 4 + m:h * 4 + m + 1]
                so = 16 + par * 4
                s1 = stt[:, so:so + 1]
                s2 = stt[:, so + 1:so + 2]
                ssn = stt[:, so + 2:so + 3]
                kst = [("mls", par)]
                if stage == 1:
                    for dc in range(2):
                        P.add("pe", lambda e, dc=dc, h=h, ms=ms, pST=pST: e.matmul(
                            pST, lhsT=kT[:, h, dc, ms], rhs=qT[:, h, dc, ms], start=(dc == 0), stop=(dc == 1)),
                            reads=k_kT(h, dc) + k_qT(h, dc), writes=psk(0))
                    P.add("dve", lambda e, par=par, pST=pST, w1c=w1c: e.scalar_tensor_tensor(
                        out=SpT[:, par, :], in0=pST, scalar=w1c, in1=cmask[:, :], op0=ALU.mult, op1=ALU.mult),
                        reads=psk(0) + ["cols", "cmask"], writes=[("SpT", par)])
                    for dc in range(2):
                        P.add("pe", lambda e, dc=dc, h=h, ms=ms, pKT=pKT: e.transpose(
                            out=pKT[:, dc * 128:(dc + 1) * 128], in_=kT[:, h, dc, ms], identity=idt[:]),
                            reads=k_kT(h, dc) + ["idt"], writes=psk(1))
                    P.add("act", lambda e, par=par, pKT=pKT, w1c=w1c: e.activation(
                        out=ktil[:, par, :], in_=pKT, func=AF.Copy, scale=w1c),
                        reads=psk(1) + ["cols"], writes=[("ktil", par)])
                if stage == 2:
                    P.add("pe", lambda e, par=par, m=m, h=h, pN=pN: e.matmul(
                        pN, lhsT=SpT[:, par, :], rhs=vext[:, m, h, 0:257], start=True, stop=False),
                        reads=[("SpT", par)] + k_v(m, h), writes=psk(3))
                    for dc in range(2):
                        P.add("pe", lambda e, dc=dc, h=h, ms=ms, pN=pN: e.matmul(
                            pN, lhsT=qT[:, h, dc, ms], rhs=Gbf[:, h, dc, 0:257], start=False, stop=(dc == 1)),
                            reads=k_qT(h, dc) + ["Gbf_%d" % h], writes=psk(3))
                    P.add("act", lambda e, s1=s1, pN=pN: e.activation(out=s1, in_=pN[:, 256:257], func=AF.Abs),
                          reads=psk(3), writes=kst)
                    P.add("dve", lambda e, s1=s1, enc=enc: e.tensor_tensor(out=s1, in0=s1, in1=enc, op=ALU.max),
                          reads=kst + ["cols"], writes=kst)
                    P.add("act", lambda e, ssn=ssn, pN=pN: e.activation(
                        out=junk_s[:, 0:256], in_=pN[:, 0:256], func=AF.Square, accum_out=ssn),
                        reads=psk(3), writes=["junk_s", ("ssn", par)])
                    P.add("dve", lambda e, s1=s1, s2=s2: e.tensor_tensor(out=s2, in0=s1, in1=s1, op=ALU.mult),
                          reads=kst, writes=kst)
                    P.add("dve", lambda e, s2=s2, ssn=ssn: e.scalar_tensor_tensor(
                        out=s2, in0=s2, scalar=256.0 * EPS, in1=ssn, op0=ALU.mult, op1=ALU.add),
                        reads=kst + [("ssn", par)], writes=kst)
                    P.add("act", lambda e, s2=s2: e.activation(out=s2, in_=s2, func=AF.Sqrt, scale=1.0 / 256.0),
                          reads=kst, writes=kst)
                    P.add("dve", lambda e, s2=s2: e.reciprocal(out=s2, in_=s2), reads=kst, writes=kst)
                    P.add("dve", lambda e, par=par, h=h, s2=s2, pN=pN: e.scalar_tensor_tensor(
                        out=hn[:, par, :], in0=pN[:, 0:256], scalar=s2, in1=ghead[:, h * 256:(h + 1) * 256],
                        op0=ALU.mult, op1=ALU.mult),
                        reads=psk(3) + kst + ["ghead"], writes=[("hn", par)])
                    for dc in range(2):
                        P.add("pe", lambda e, dc=dc, par=par, m=m, h=h, pP=pP: e.matmul(
                            pP[dc], lhsT=ktil[:, par, dc * 128:(dc + 1) * 128], rhs=vext[:, m, h, 0:257],
                            start=True, stop=True),
                            reads=[("ktil", par)] + k_v(m, h), writes=psk(4 + dc))
                    P.add("act", lambda e, par=par, h=h, dcc=dcc: e.activation(
                        out=Gs[:, par, :, :], in_=G32[:, h, :, :], func=AF.Copy, scale=dcc),
                        reads=["G32_%d" % h, "decbc"], writes=[("Gs", par)])
                    for dc in range(2):
                        P.add("dve", lambda e, dc=dc, par=par, h=h, dcc=dcc, pP=pP: e.scalar_tensor_tensor(
                            out=G32[:, h, dc, :], in0=pP[dc], scalar=dcc, in1=Gs[:, par, dc, :],
                            op0=ALU.mult, op1=ALU.add),
                            reads=psk(4 + dc) + [("Gs", par), "decbc"], writes=["G32_%d" % h])
                    P.add("act", lambda e, h=h: e.activation(out=Gbf[:, h, :, 0:257], in_=G32[:, h, :, :], func=AF.Copy),
                          reads=["G32_%d" % h], writes=["Gbf_%d" % h])
                if stage == 3:
                    for ec in range(2):
                        P.add("pe", lambda e, ec=ec, par=par, pHT=pHT: e.transpose(
                            out=pHT[:, ec * 128:(ec + 1) * 128], in_=hn[:, par, ec * 128:(ec + 1) * 128], identity=idt[:]),
                            reads=[("hn", par), "idt"], writes=psk(2))
                    P.add("dve", lambda e, h=h, ms=ms, pHT=pHT: e.tensor_tensor(
                        out=yT[:, 8 + 2 * h:10 + 2 * h, ms], in0=pHT.rearrange("p (c t) -> p c t", c=2),
                        in1=soT[:, h, :, ms], op=ALU.mult),
                        reads=psk(2) + k_soT(h, 0) + k_soT(h, 1), writes=[("R", 8 + 2 * h), ("R", 9 + 2 * h)])

            steps = [(m, h) for m in range(4) for h in range(4)]
            conv_calls = o_calls + conv_calls
            cci = 0
            for it in range(len(steps) + 2):
                if it < len(steps):
                    ml_step(steps[it][0], steps[it][1], 1)
                if 0 <= it - 1 < len(steps):
                    ml_step(steps[it - 1][0], steps[it - 1][1], 2)
                if 0 <= it - 2 < len(steps):
                    ml_step(steps[it - 2][0], steps[it - 2][1], 3)
                if cci < len(conv_calls):
                    conv_calls[cci]()
                    cci += 1
            while cci < len(conv_calls):
                conv_calls[cci]()
                cci += 1
            if dbg and b == 0:
                P.add("sp", lambda e: e.dma_start(out=d_yT, in_=yT[:, :, :].rearrange("p k t -> p (k t)")),
                      reads=[("R", k) for k in range(16)], dma="d_yT")

            for n in range(4):
                tm_proj(w_out, n * 512, yT, lambda k: [("R", k)], 16, post_evac(n, 24, 0), wname="w_out",
                        bank0=4 * (n % 2))
            rss = [post_rstd(m, 24) for m in range(4)]
            for m in range(4):
                P.add("dve", lambda e, m=m, rs=rss[m]: e.scalar_tensor_tensor(
                    out=xres[:, m, :], in0=y2s[:, m, :], scalar=rs, in1=xres[:, m, :], op0=ALU.mult, op1=ALU.add),
                    reads=k_y2all(m) + [("rsp", m), ("xres", m)], writes=[("xres", m)])
            if not (dbg and b == 0):
                for m in range(4):
                    norm_pre_sq(m)
                for m in range(4):
                    norm_pre_rs(m)
                for m in range(4):
                    norm_pre_scale(m)
            if dbg and b == 0:
                P.add("sp", lambda e: e.dma_start(out=d_x1, in_=xres[:, :, :].rearrange("p m d -> p (m d)")),
                      reads=[("xres", m) for m in range(4)], dma="d_x1")
            if dbg and b == 0:
                for m in range(4):
                    norm_pre(m)
            norm_tr(bufA, k_uT, 16, (0, 1))
            norm_tr(bufA, k_uT, 16, (2, 3))

            for fp in range(32):
                def ev_h(ci, bank, bkey, fp=fp):
                    f = 2 * fp + ci
                    P.add("act", lambda e: e.activation(out=rbuf[:, ci, :], in_=bank[:, :], func=AF.Relu),
                          reads=bkey, writes=[("rbuf", ci)])
                    P.add("dve", lambda e: e.tensor_tensor(out=hT[:, f, :], in0=rbuf[:, ci, :], in1=rbuf[:, ci, :],
                                                           op=ALU.mult),
                          reads=[("rbuf", ci)], writes=[("R", f)])
                fm_proj(w1, fp * 256, bufA, k_uT, ev_h, wname="w1")
                if b + 1 < nblk and fp >= 3 and (fp - 3) % 6 == 0 and (fp - 3) // 6 < 4:
                    stageA_part1(b + 1, (fp - 3) // 6)

            for n in range(4):
                tm_proj(w2, n * 512, hT, lambda k: [("R", k)], 64, post_evac(n, 40, 1), wname="w2", bank0=4 * (n % 2))
                if b + 1 < nblk and n == 1:
                    stageA_part2(0)
            rss = [post_rstd(m, 40) for m in range(4)]
            for m in range(4):
                P.add("dve", lambda e, m=m, rs=rss[m]: e.scalar_tensor_tensor(
                    out=y2s[:, m, :], in0=y2s[:, m, :], scalar=rs, in1=xres[:, m, :], op0=ALU.mult, op1=ALU.add),
                    reads=k_y2all(m) + [("rsp", m), ("xres", m)], writes=k_y2all(m))
                r0 = t0 + m * 128
                P.add(mq, lambda e, m=m, r0=r0: e.dma_start(out=out[r0:r0 + 128, :], in_=y2s[:, m, :]),
                      reads=k_y2all(m), dma=("xout", m))

        P.emit(st)
    return nc


def host_consts(inp):
    g_pre_mix = np.asarray(inp["g_pre_mix"], np.float32)[0]
    g_pre_mlp = np.asarray(inp["g_pre_mlp"], np.float32)[0]
    gcol = np.concatenate([g_pre_mix.reshape(16, 128).T, g_pre_mlp.reshape(16, 128).T], axis=1)
    gpost = np.stack([np.broadcast_to(np.asarray(inp["g_post_mix"], np.float32)[0], (128, D)),
                      np.broadcast_to(np.asarray(inp["g_post_mlp"], np.float32)[0], (128, D))], axis=1)
    ghead = np.broadcast_to(np.asarray(inp["g_head"], np.float32)[0], (128, 1024))
    convw = np.asarray(inp["conv_w"], np.float32)[0].reshape(3, 8, 128).transpose(2, 1, 0)
    bif = np.stack([np.asarray(inp["b_i"], np.float32)[0], np.asarray(inp["b_f"], np.float32)[0]], axis=1)
    ident = np.eye(128, dtype=np.float32).astype(ml_dtypes.bfloat16)
    cmask = np.triu(np.ones((128, 128), np.float32))
    sel = np.zeros((4, 512), np.float32)
    for h in range(4):
        sel[h, h * 128:(h + 1) * 128] = 1.0
    i4 = np.eye(4, dtype=np.float32)
    c = dict(gcol=gcol, gpost=gpost, ghead=ghead, convw=convw, bif=bif, ident=ident, cmask=cmask, sel=sel, i4=i4)
    return {k: np.ascontiguousarray(v) for k, v in c.items()}


def kernel(**inputs):
    n = 8
    x = np.asarray(inputs["x"], np.float32)
    shared = host_consts(inputs)
    shared["w_in"] = np.ascontiguousarray(np.asarray(inputs["w_in"], np.float32)[0])
    shared["w_out"] = np.ascontiguousarray(np.asarray(inputs["w_out"], np.float32)[0])
    shared["w_mlp1"] = np.ascontiguousarray(np.asarray(inputs["w_mlp1"], np.float32)[0])
    shared["w_mlp2"] = np.ascontiguousarray(np.asarray(inputs["w_mlp2"], np.float32)[0])
    nc = build_nc()
    in_maps = []
    for c in range(n):
        d = dict(shared)
        d["x"] = np.ascontiguousarray(x[c])
        in_maps.append(d)
    res = run_bass_kernel_spmd(nc, in_maps, core_ids=list(range(n)))
    return np.stack([np.asarray(r["out"], np.float32) for r in res.results], axis=0)
```
